# Optimizing a Trainium2 kernel written in Bass

```python
import jax, jax.numpy as jnp
from jax import lax
import numpy as np

D_MODEL = 1024
BATCH = 1
SEQ = 16384
DEPTH = 4

HEAD_DIM = 64
NSA_HEADS = 8
NSA_KV_GROUPS = 1
NSA_HPG = NSA_HEADS // NSA_KV_GROUPS
SB_HEADS = 4
SB_HEAD_DIM = 128
ROPE_THETA = 500000.0
ROT_DIM = HEAD_DIM // 4
CMP_BLOCK = 32
CMP_STRIDE = 16
CMP_HIDDEN = 256
SEL_BLOCK = 64
SEL_TOP_N = 8
WINDOW = 512
Q_BLOCK = 128
D_FF = 4 * D_MODEL
RMS_EPS = 1e-6
NSA_W = NSA_HEADS * HEAD_DIM
KV_W = NSA_KV_GROUPS * HEAD_DIM
SB_W = SB_HEADS * SB_HEAD_DIM
IN_W = NSA_W + 6 * KV_W + 3 * NSA_HEADS + 3 * SB_W + 2 * D_MODEL

kernel_name = 'hybrid_nsa_stickbreaking_sqrelu_block'


def rms_norm(x, g):
    xf = x.astype(jnp.float32)
    y = xf * lax.rsqrt(jnp.mean(xf * xf, axis=-1, keepdims=True) + RMS_EPS)
    return (y * g.astype(jnp.float32)).astype(x.dtype)


def to_heads(t, n, dh):
    b, s = t.shape[:2]
    return t.reshape(b, s, n, dh).transpose(0, 2, 1, 3)


def partial_rope(x, positions):
    half = ROT_DIM // 2
    inv_freq = jnp.power(ROPE_THETA, jnp.arange(half, dtype=jnp.float32) * (-2.0 / ROT_DIM))
    ang = positions.astype(jnp.float32)[:, None, :, None] * inv_freq
    cos, sin = jnp.cos(ang), jnp.sin(ang)
    xf = x.astype(jnp.float32)
    x1, x2, rest = xf[..., :half], xf[..., half:ROT_DIM], xf[..., ROT_DIM:]
    out = jnp.concatenate([x1 * cos - x2 * sin, x2 * cos + x1 * sin, rest], axis=-1)
    return out.astype(x.dtype)


def compress_blocks(kv, pe, w1, w2):
    s = kv.shape[2]
    nc = (s - CMP_BLOCK) // CMP_STRIDE + 1
    idx = CMP_STRIDE * jnp.arange(nc)[:, None] + jnp.arange(CMP_BLOCK)[None, :]
    blocks = kv[:, :, idx] + pe
    flat = blocks.reshape(blocks.shape[:3] + (CMP_BLOCK * HEAD_DIM,))
    return jax.nn.gelu(flat @ w1) @ w2


def nsa_attention(q_rope, q_plain, k_cmp, v_cmp, k_slc, v_slc, k_win, v_win, gates):
    out_dtype = q_rope.dtype
    f32 = jnp.float32
    scale = HEAD_DIM ** -0.5
    q_rope, q_plain, gates = q_rope.astype(f32) * scale, q_plain.astype(f32) * scale, gates.astype(f32)
    k_cmp, v_cmp = k_cmp.astype(f32), v_cmp.astype(f32)
    b, g, h, s, dh = q_rope.shape
    nb = s // Q_BLOCK
    nc = k_cmp.shape[2]
    ns = s // SEL_BLOCK
    n_sel = min(SEL_TOP_N, ns)
    cmp_last = CMP_STRIDE * jnp.arange(nc) + CMP_BLOCK - 1
    c_start = CMP_STRIDE * jnp.arange(nc)[:, None]
    s_start = SEL_BLOCK * jnp.arange(ns)[None, :]
    overlap = ((c_start < s_start + SEL_BLOCK) & (c_start + CMP_BLOCK > s_start)).astype(f32)
    ks_blocks = k_slc.astype(f32).reshape(b, g, ns, SEL_BLOCK, dh)
    vs_blocks = v_slc.astype(f32).reshape(b, g, ns, SEL_BLOCK, dh)
    pad = ((0, 0), (0, 0), (WINDOW, 0), (0, 0))
    kw_pad = jnp.pad(k_win.astype(f32), pad)
    vw_pad = jnp.pad(v_win.astype(f32), pad)
    b_ix = jnp.arange(b)[:, None, None, None]
    g_ix = jnp.arange(g)[None, :, None, None]
    blk = jnp.arange(ns)

    def block(i):
        q0 = i * Q_BLOCK
        t = q0 + jnp.arange(Q_BLOCK)
        qr = lax.dynamic_slice_in_dim(q_rope, q0, Q_BLOCK, axis=3)
        qp = lax.dynamic_slice_in_dim(q_plain, q0, Q_BLOCK, axis=3)
        gt = lax.dynamic_slice_in_dim(gates, q0, Q_BLOCK, axis=3)
        vis_c = cmp_last[None, :] <= t[:, None]
        s_c = jnp.einsum('bghqd,bgcd->bghqc', qp, k_cmp)
        p_c = jax.nn.softmax(jnp.where(vis_c, s_c, -1e30), axis=-1) * vis_c.astype(f32)
        o_c = jnp.einsum('bghqc,bgcd->bghqd', p_c, v_cmp)
        imp = jnp.einsum('bghqc,cn->bgqn', p_c, overlap)
        cur = t // SEL_BLOCK
        valid = blk[None, :] <= cur[:, None]
        forced = (blk[None, :] == 0) | (blk[None, :] == cur[:, None]) | (blk[None, :] == cur[:, None] - 1)
        score = jnp.where(valid, jnp.where(forced, jnp.inf, imp), -1.0)
        _, sel = lax.top_k(score, n_sel)
        k_g = ks_blocks[b_ix, g_ix, sel].reshape(b, g, Q_BLOCK, n_sel * SEL_BLOCK, dh)
        v_g = vs_blocks[b_ix, g_ix, sel].reshape(b, g, Q_BLOCK, n_sel * SEL_BLOCK, dh)
        pos = sel[..., None] * SEL_BLOCK + jnp.arange(SEL_BLOCK)
        vis_s = (pos <= t[:, None, None]).reshape(b, g, Q_BLOCK, n_sel * SEL_BLOCK)
        s_s = jnp.einsum('bghqd,bgqkd->bghqk', qr, k_g)
        p_s = jax.nn.softmax(jnp.where(vis_s[:, :, None], s_s, -1e30), axis=-1)
        o_s = jnp.einsum('bghqk,bgqkd->bghqd', p_s, v_g)
        kw = lax.dynamic_slice_in_dim(kw_pad, q0, Q_BLOCK + WINDOW, axis=2)
        vw = lax.dynamic_slice_in_dim(vw_pad, q0, Q_BLOCK + WINDOW, axis=2)
        kpos = q0 - WINDOW + jnp.arange(Q_BLOCK + WINDOW)
        vis_w = (kpos[None, :] <= t[:, None]) & (kpos[None, :] > t[:, None] - WINDOW) & (kpos[None, :] >= 0)
        s_w = jnp.einsum('bghqd,bgkd->bghqk', qr, kw)
        p_w = jax.nn.softmax(jnp.where(vis_w, s_w, -1e30), axis=-1)
        o_w = jnp.einsum('bghqk,bgkd->bghqd', p_w, vw)
        return gt[..., 0:1] * o_c + gt[..., 1:2] * o_s + gt[..., 2:3] * o_w

    out = lax.map(block, jnp.arange(nb))
    out = out.transpose(1, 0, 4, 2, 3, 5).reshape(b, s, g * h * dh)
    return out.astype(out_dtype)


def stick_breaking_attention(q, k, v):
    out_dtype = q.dtype
    f32 = jnp.float32
    b, hh, s, dh = q.shape
    qf = q.astype(f32) * (SB_HEAD_DIM ** -0.5)
    kf, vf = k.astype(f32), v.astype(f32)
    nb = s // Q_BLOCK
    r = jnp.arange(Q_BLOCK)
    tri = (r[:, None] >= r[None, :]).astype(f32)
    diag_mask = r[None, :] < r[:, None]
    outs = []
    for i in range(nb):
        q0 = i * Q_BLOCK
        qb = qf[:, :, q0:q0 + Q_BLOCK]
        z_d = jnp.einsum('bhqd,bhkd->bhqk', qb, kf[:, :, q0:q0 + Q_BLOCK])
        l_d = jnp.where(diag_mask, jax.nn.log_sigmoid(-z_d), 0.0)
        c_d = l_d @ tri
        a_d = jnp.where(diag_mask, jnp.exp(z_d + c_d), 0.0)
        o = jnp.einsum('bhqk,bhkd->bhqd', a_d, vf[:, :, q0:q0 + Q_BLOCK])
        if i > 0:
            z_o = jnp.einsum('bhqd,bhkd->bhqk', qb, kf[:, :, :q0]).reshape(b, hh, Q_BLOCK, i, Q_BLOCK)
            c_o = jax.nn.log_sigmoid(-z_o) @ tri
            ci = jnp.arange(i)
            upper = (ci[:, None] > ci[None, :]).astype(f32)
            off = c_o[..., 0] @ upper + c_d[..., 0:1]
            a_o = jnp.exp(z_o + c_o + off[..., None])
            o = o + jnp.einsum('bhqck,bhckd->bhqd', a_o, vf[:, :, :q0].reshape(b, hh, i, Q_BLOCK, dh))
        outs.append(o)
    out = jnp.concatenate(outs, axis=2)
    return out.transpose(0, 2, 1, 3).reshape(b, s, hh * dh).astype(out_dtype)


def split_columns(proj):
    sizes = [NSA_W, KV_W, KV_W, KV_W, KV_W, KV_W, KV_W, 3 * NSA_HEADS, SB_W, SB_W, SB_W, 2 * D_MODEL]
    parts, start = [], 0
    for n in sizes:
        parts.append(proj[..., start:start + n])
        start += n
    return parts


def hybrid_layer(x, positions, norm_g, w_in, cmp_pe, cmp_w1, cmp_w2, w_nsa_o, w_sb_o, w_out, w_ff1, w_ff2):
    b, s, _ = x.shape
    hn = rms_norm(x, norm_g[0])
    (nsa_q, k_c, v_c, k_s, v_s, k_w, v_w, nsa_g, sb_q, sb_k, sb_v, merge_g) = split_columns(hn @ w_in)
    grp = (b, NSA_KV_GROUPS, NSA_HPG, s, HEAD_DIM)
    q_plain = to_heads(nsa_q, NSA_HEADS, HEAD_DIM)
    q_rope = partial_rope(q_plain, positions).reshape(grp)
    q_plain = q_plain.reshape(grp)
    kc = compress_blocks(to_heads(k_c, NSA_KV_GROUPS, HEAD_DIM), cmp_pe[0], cmp_w1[0], cmp_w2[0])
    vc = compress_blocks(to_heads(v_c, NSA_KV_GROUPS, HEAD_DIM), cmp_pe[1], cmp_w1[1], cmp_w2[1])
    ks = partial_rope(to_heads(k_s, NSA_KV_GROUPS, HEAD_DIM), positions)
    kw = partial_rope(to_heads(k_w, NSA_KV_GROUPS, HEAD_DIM), positions)
    gates = jax.nn.sigmoid(nsa_g).reshape(b, s, NSA_HEADS, 3).transpose(0, 2, 1, 3).reshape(grp[:4] + (3,))
    y_nsa = nsa_attention(q_rope, q_plain, kc, vc, ks, to_heads(v_s, NSA_KV_GROUPS, HEAD_DIM),
                          kw, to_heads(v_w, NSA_KV_GROUPS, HEAD_DIM), gates) @ w_nsa_o
    y_sb = stick_breaking_attention(to_heads(sb_q, SB_HEADS, SB_HEAD_DIM), to_heads(sb_k, SB_HEADS, SB_HEAD_DIM),
                                    to_heads(sb_v, SB_HEADS, SB_HEAD_DIM)) @ w_sb_o
    gate = jax.nn.sigmoid(merge_g)
    mixed = (gate[..., :D_MODEL] * y_nsa + gate[..., D_MODEL:] * y_sb) @ w_out
    x = x + rms_norm(mixed, norm_g[1])
    hf = rms_norm(x, norm_g[2])
    ff = jnp.square(jax.nn.relu(hf @ w_ff1)) @ w_ff2
    return x + rms_norm(ff, norm_g[3])


def setup_inputs(seed: int = 0) -> dict:
    key = jax.random.key(seed)
    ks = jax.random.split(key, 12)
    f32 = jnp.float32

    def nrm(k, shape, fan_in):
        return jax.random.normal(k, shape, f32) * (fan_in ** -0.5)

    x = jax.random.normal(ks[0], (BATCH, SEQ, D_MODEL), f32)
    positions = jnp.broadcast_to(jnp.arange(SEQ, dtype=jnp.int32)[None, :], (BATCH, SEQ))
    norm_g = 1.0 + 0.05 * jax.random.normal(ks[1], (DEPTH, 4, D_MODEL), f32)
    w_in = nrm(ks[2], (DEPTH, D_MODEL, IN_W), D_MODEL)
    cmp_pe = 0.5 * jax.random.normal(ks[3], (DEPTH, 2, CMP_BLOCK, HEAD_DIM), f32)
    cmp_w1 = nrm(ks[4], (DEPTH, 2, CMP_BLOCK * HEAD_DIM, CMP_HIDDEN), CMP_BLOCK * HEAD_DIM)
    cmp_w2 = nrm(ks[5], (DEPTH, 2, CMP_HIDDEN, HEAD_DIM), CMP_HIDDEN)
    w_nsa_o = nrm(ks[6], (DEPTH, NSA_W, D_MODEL), NSA_W)
    w_sb_o = nrm(ks[7], (DEPTH, SB_W, D_MODEL), SB_W)
    w_out = nrm(ks[8], (DEPTH, D_MODEL, D_MODEL), D_MODEL)
    w_ff1 = nrm(ks[9], (DEPTH, D_MODEL, D_FF), D_MODEL)
    w_ff2 = nrm(ks[10], (DEPTH, D_FF, D_MODEL), D_FF)
    return {'x': x, 'positions': positions, 'norm_g': norm_g, 'w_in': w_in, 'cmp_pe': cmp_pe,
            'cmp_w1': cmp_w1, 'cmp_w2': cmp_w2, 'w_nsa_o': w_nsa_o, 'w_sb_o': w_sb_o,
            'w_out': w_out, 'w_ff1': w_ff1, 'w_ff2': w_ff2}


def reference(x, positions, norm_g, w_in, cmp_pe, cmp_w1, cmp_w2, w_nsa_o, w_sb_o, w_out, w_ff1, w_ff2):
    for layer in range(DEPTH):
        x = hybrid_layer(x, positions, norm_g[layer], w_in[layer], cmp_pe[layer], cmp_w1[layer],
                         cmp_w2[layer], w_nsa_o[layer], w_sb_o[layer], w_out[layer],
                         w_ff1[layer], w_ff2[layer])
    return x
```

```python
import numpy as np
import concourse.bass as bass
import concourse.mybir as mybir
from concourse.bass_utils import run_bass_kernel_spmd

F32 = mybir.dt.float32
BF16 = mybir.dt.bfloat16
I32 = mybir.dt.int32
AF = mybir.ActivationFunctionType
ALU = mybir.AluOpType

NCORES = 8
S = 16384
D = 1024
DEPTH = 4
TOK = S // NCORES
NSB = TOK // 512
DFF = 4096
IN_W = 4504
EPS = 1e-6
NEG = -30000.0
TWO_PI = float(2 * np.pi)

SEM_LIMIT = 30000
import os
STORE_ENG = os.environ.get("STORE_ENG", "act")
CAST_ENG = os.environ.get("CAST_ENG", "pool")
ATTACH_WAITS = os.environ.get("ATTACH_WAITS", "1") == "1"
ENGS = ("pe", "act", "dve", "pool", "sp")


class Dep:
    __slots__ = ("w", "r", "dsem", "dcnt")

    def __init__(self):
        self.w = None
        self.r = []
        self.dsem = None
        self.dcnt = 0


class Op:
    __slots__ = ("eng", "fn", "reads", "writes", "dma", "waits", "sig", "semid", "semval", "dsem", "dval", "bar")

    def __init__(self, eng, fn, reads, writes, dma):
        self.eng, self.fn, self.reads, self.writes, self.dma = eng, fn, reads, writes, dma
        self.waits = ()
        self.sig = False
        self.semid = None
        self.semval = 0
        self.dsem = None
        self.dval = 0
        self.bar = False


class _Rec:
    def __getattr__(self, name):
        return lambda *a, **k: (name, a, k)


_REC = _Rec()


class Prog:
    def __init__(self, nc):
        self.nc = nc
        self.ops = []
        self.engs = {"pe": nc.tensor, "act": nc.scalar, "dve": nc.vector, "pool": nc.gpsimd, "sp": nc.sync}
        self.n_dsem = 0
        self.dma_rr = 0
        self.stack = None

    def sb(self, name, shape, dtype):
        if self.stack is not None:
            return self.stack.enter_context(self.nc.sbuf_tensor("s_" + name, list(shape), dtype))
        return self.nc.alloc_sbuf_tensor("s_" + name, list(shape), dtype)

    def open_scope(self):
        import contextlib
        assert self.stack is None
        self.stack = contextlib.ExitStack()

    def close_scope(self):
        self.barrier()
        self.stack.close()
        self.stack = None

    def ps(self, name, shape, dtype=F32):
        return self.nc.alloc_psum_tensor("p_" + name, list(shape), dtype)

    def op(self, eng, fn, reads=(), writes=()):
        self.ops.append(Op(eng, fn(_REC), tuple(reads), tuple(writes), False))

    def dma(self, out, in_, reads=(), writes=(), prim=None, eng=None, **kw):
        if eng is None:
            eng = "sp"
        if prim is None:
            prim = (writes[0] if writes else reads[0])
        if prim.dsem is None:
            prim.dsem = self.n_dsem
            self.n_dsem += 1
        o = Op(eng, (out, in_, kw), tuple(reads), tuple(writes), True)
        prim.dcnt += 16
        o.dsem, o.dval = prim.dsem, prim.dcnt
        self.ops.append(o)

    def barrier(self):
        for e in ENGS:
            o = Op(e, None, (), (), False)
            o.bar = True
            self.ops.append(o)

    def finalize(self, final_deps=()):
        nc = self.nc
        ops = self.ops
        last_eng = {e: None for e in ENGS}
        pend_dma = []
        bar_need = None
        for i, o in enumerate(ops):
            if o.bar:
                if bar_need is None:
                    bar_need = set(j for j in last_eng.values() if j is not None) | set(pend_dma)
                o.waits = set(bar_need)
                continue
            if bar_need is not None:
                bar_need = None
                pend_dma = []
            need = set()
            for d in o.reads:
                if d.w is not None:
                    need.add(d.w)
            for d in o.writes:
                if d.w is not None:
                    need.add(d.w)
                for r in d.r:
                    need.add(r)
            need.discard(i)
            if o.dma:
                need = set(j for j in need if not (ops[j].dma and ops[j].dsem == o.dsem))
            o.waits = need
            for d in o.reads:
                d.r.append(i)
            for d in o.writes:
                d.w = i
                d.r = []
            if o.dma:
                pend_dma.append(i)
            else:
                last_eng[o.eng] = i
        fin_need = set()
        for d in final_deps:
            if d.w is not None:
                fin_need.add(d.w)
            for r in d.r:
                fin_need.add(r)
        for o in ops:
            for j in o.waits:
                if not ops[j].dma:
                    ops[j].sig = True
        for j in fin_need:
            if not ops[j].dma:
                ops[j].sig = True
        cnt = {e: 0 for e in ENGS}
        for o in ops:
            if o.sig and not o.dma:
                k = cnt[o.eng]
                cnt[o.eng] += 1
                o.semid = (o.eng, k // SEM_LIMIT)
                o.semval = (k % SEM_LIMIT) + 1
        sems = {}
        for e in ENGS:
            for ep in range((cnt[e] + SEM_LIMIT - 1) // SEM_LIMIT):
                sems[(e, ep)] = nc.alloc_semaphore(f"s_{e}_{ep}")
        dsems = [nc.alloc_semaphore(f"d_{i}") for i in range(self.n_dsem)]
        waited = {e: {} for e in ENGS}
        n_wait = 0

        def collect_waits(eng, need):
            wl = {}
            for j in need:
                p = ops[j]
                if p.dma:
                    key, val = ("d", p.dsem), p.dval
                else:
                    key, val = p.semid, p.semval
                if wl.get(key, 0) < val:
                    wl[key] = val
            res = []
            for key, val in wl.items():
                if waited[eng].get(key, 0) >= val:
                    continue
                waited[eng][key] = val
                res.append((dsems[key[1]] if key[0] == "d" else sems[key], val))
            return res

        for o in ops:
            E = self.engs[o.eng]
            wl = collect_waits(o.eng, o.waits)
            attach = None
            if wl and not o.bar and ATTACH_WAITS:
                attach = wl.pop()
            for s_, v_ in wl:
                E.wait_ge(s_, v_)
                n_wait += 1
            if o.bar:
                continue
            if o.dma:
                out, in_, kw = o.fn
                ins = E.dma_start(out=out, in_=in_, **kw)
                if attach is not None:
                    ins._wait_ge(attach[0], attach[1])
                ins.then_inc(dsems[o.dsem], 16)
            else:
                ins = getattr(E, o.fn[0])(*o.fn[1], **o.fn[2])
                if attach is not None:
                    ins._wait_ge(attach[0], attach[1])
                if o.sig:
                    ins.then_inc(sems[o.semid], 1)
        for s_, v_ in collect_waits("sp", fin_need):
            self.engs["sp"].wait_ge(s_, v_)
        self.stats = dict(n_ops=len(ops), n_wait=n_wait, n_sems=len(sems) + len(dsems))
        return self.stats


class Ring:
    def __init__(self, P, name, n, shape, dtype, psum=False):
        self.tiles = [(P.ps if psum else P.sb)(f"{name}{i}", shape, dtype) for i in range(n)]
        self.deps = [Dep() for _ in range(n)]
        self.i = 0
        self.n = n

    def next(self):
        t, d = self.tiles[self.i], self.deps[self.i]
        self.i = (self.i + 1) % self.n
        return t, d


def dram_in(nc, name, shape, dt):
    return nc.dram_tensor(name, list(shape), dt, kind="ExternalInput").ap()


def dram_out(nc, name, shape, dt):
    return nc.dram_tensor(name, list(shape), dt, kind="ExternalOutput").ap()


def _swap_cols(base):
    c = np.arange(base, base + 64)
    o = c.copy()
    o[0:8] = c[8:16]
    o[8:16] = c[0:8]
    return o


def win_col_groups():
    g = []
    for i in range(4):
        g.append(np.arange(128 * i, 128 * i + 128))
    for i in range(4):
        g.append(np.concatenate([_swap_cols(128 * i), _swap_cols(128 * i + 64)]))
    g.append(np.arange(512, 640))
    g.append(np.concatenate([np.arange(640, 704), np.arange(768, 832)]))
    g.append(np.concatenate([_swap_cols(640), _swap_cols(768)]))
    for i in range(12):
        g.append(np.concatenate([np.full(64, 896 + 2 * i), np.full(64, 896 + 2 * i + 1)]))
    for i in range(4):
        g.append(np.arange(920 + 128 * i, 920 + 128 * i + 128))
    for i in range(4):
        g.append(np.arange(1432 + 128 * i, 1432 + 128 * i + 128))
    for i in range(16):
        g.append(np.arange(2456 + 128 * i, 2456 + 128 * i + 128))
    return g


NG_FM = 47
TM_COLS = np.concatenate([np.arange(704, 768), np.arange(832, 896), np.arange(1944, 2456)])


def layout_w_in(w):
    groups = win_col_groups()
    fm = np.stack([w[:, c].reshape(8, 128, 128).transpose(1, 0, 2) for c in groups])
    tm = w[:, TM_COLS].reshape(8, 128, 640).transpose(1, 0, 2)
    return np.ascontiguousarray(fm), np.ascontiguousarray(tm)


def rope_consts():
    half = 8
    inv = np.power(np.float32(500000.0), np.arange(half, dtype=np.float32) * np.float32(-2.0 / 16)).astype(np.float32)
    c = np.zeros((128, 2), np.float32)
    for p in range(128):
        d = p % 64
        if d < 8:
            c[p, 0] = inv[d]
            c[p, 1] = -1.0
        elif d < 16:
            c[p, 0] = inv[d - 8]
            c[p, 1] = 1.0
    return c


def emit_rms_rstd(P, ss, dss, rstd, drstd, n):
    P.op("dve", lambda E: E.tensor_scalar(out=rstd[:, 0:n], in0=ss[:, 0:n], scalar1=1.0 / D, scalar2=EPS,
                                          op0=ALU.mult, op1=ALU.add), reads=[dss], writes=[drstd])
    P.op("act", lambda E: E.activation(out=rstd[:, 0:n], in_=rstd[:, 0:n], func=AF.Sqrt), reads=[drstd], writes=[drstd])
    P.op("dve", lambda E: E.reciprocal(out=rstd[:, 0:n], in_=rstd[:, 0:n]), reads=[drstd], writes=[drstd])


def emit_sin_table(P, out, dout, ang, dang, tmp_i, tmp_f, dtmp, shift):
    P.op("dve", lambda E: E.tensor_scalar(out=out, in0=ang[:], scalar1=float(shift), scalar2=None, op0=ALU.add),
         reads=[dang], writes=[dout])
    P.op("dve", lambda E: E.tensor_scalar(out=tmp_i[:], in0=out, scalar1=1.0 / TWO_PI, scalar2=None, op0=ALU.mult),
         reads=[dout], writes=[dtmp])
    P.op("dve", lambda E: E.tensor_copy(out=tmp_f[:], in_=tmp_i[:]), reads=[dtmp], writes=[dtmp])
    P.op("dve", lambda E: E.scalar_tensor_tensor(out=out, in0=tmp_f[:], scalar=-TWO_PI, in1=out, op0=ALU.mult,
                                                 op1=ALU.add), reads=[dtmp, dout], writes=[dout])
    P.op("dve", lambda E: E.tensor_scalar(out=tmp_f[:], in0=out, scalar1=float(np.pi), scalar2=-TWO_PI,
                                          op0=ALU.is_gt, op1=ALU.mult), reads=[dout], writes=[dtmp])
    P.op("dve", lambda E: E.tensor_tensor(out=out, in0=out, in1=tmp_f[:], op=ALU.add), reads=[dout, dtmp],
         writes=[dout])
    P.op("dve", lambda E: E.tensor_scalar(out=tmp_f[:], in0=out, scalar1=float(-np.pi), scalar2=TWO_PI,
                                          op0=ALU.is_lt, op1=ALU.mult), reads=[dout], writes=[dtmp])
    P.op("dve", lambda E: E.tensor_tensor(out=out, in0=out, in1=tmp_f[:], op=ALU.add), reads=[dout, dtmp],
         writes=[dout])
    P.op("act", lambda E: E.activation(out=out, in_=out, func=AF.Sin), reads=[dout], writes=[dout])


def emit_norm_transpose(P, xt, dx, a, rstd, drstd, gbc, dg, hb_ring, ident, dident, ptr, dptr, hT, dhT):
    hb, dhb = hb_ring.next()
    P.op("dve", lambda E: E.scalar_tensor_tensor(out=hb[:], in0=xt[:, a, :], scalar=rstd[:, a:a + 1], in1=gbc[:],
                                                 op0=ALU.mult, op1=ALU.mult), reads=[dx, drstd, dg], writes=[dhb])
    for kc in range(8):
        P.op("pe", lambda E, kc=kc: E.transpose(ptr[:, kc * 128:(kc + 1) * 128], hb[:, kc * 128:(kc + 1) * 128], ident[:]),
             reads=[dhb, dident], writes=[dptr])
    P.op("act", lambda E: E.activation(out=hT[:, :, a * 128:(a + 1) * 128],
                                       in_=ptr[:].rearrange("p (k t) -> p k t", k=8), func=AF.Copy),
         reads=[dptr], writes=[dhT])


def build_phase_a(dbg=0):
    nc = bass.Bass("TRN2", target_bir_lowering=False)
    x = dram_in(nc, "x", [TOK, D], F32)
    pos = dram_in(nc, "pos", [128, TOK], I32)
    gbc_in = dram_in(nc, "gbc", [128, D], F32)
    rc_in = dram_in(nc, "rc", [128, 2], F32)
    ident_in = dram_in(nc, "ident", [128, 128], F32)
    wfm = dram_in(nc, "wfm", [NG_FM, 128, 8, 128], F32)
    wtm = dram_in(nc, "wtm", [128, 8, 640], F32)
    o_qp = dram_out(nc, "qpT", [8, 64, TOK], BF16)
    o_qr = dram_out(nc, "qrT", [8, 64, TOK], BF16)
    o_kcvc = dram_out(nc, "kcvcT", [128, TOK], BF16)
    o_kskw = dram_out(nc, "kskwT", [128, TOK], BF16)
    o_g = dram_out(nc, "G", [24, 64, TOK], BF16)
    o_sq = dram_out(nc, "sqT", [4, 128, TOK], BF16)
    o_sk = dram_out(nc, "skT", [4, 128, TOK], BF16)
    o_mg = dram_out(nc, "mgT", [16, 128, TOK], BF16)
    o_vv = dram_out(nc, "vsvw", [TOK, 128], BF16)
    o_sv = dram_out(nc, "sv", [TOK, 512], BF16)
    P = Prog(nc)
    fin = []
    NT = TOK // 128

    xt = P.sb("xt", [128, NT, D], F32); dx = Dep()
    for a in range(NT):
        P.dma(xt[:, a, :], x[a * 128:(a + 1) * 128, :], writes=[dx])
    gbc = P.sb("gbc", [128, D], F32); dg = Dep()
    P.dma(gbc[:], gbc_in, writes=[dg])
    rc = P.sb("rc", [128, 2], F32); drc = Dep()
    P.dma(rc[:], rc_in, writes=[drc])
    idf = P.sb("idf", [128, 128], F32); didf = Dep()
    P.dma(idf[:], ident_in, writes=[didf])
    ident = P.sb("ident", [128, 128], BF16); dident = Dep()
    P.op("dve", lambda E: E.tensor_copy(out=ident[:], in_=idf[:]), reads=[didf], writes=[dident])
    posi = P.sb("posi", [128, TOK], I32); dposi = Dep()
    P.dma(posi[:], pos, writes=[dposi])

    ang = P.sb("ang", [128, 512], F32); dang = Dep()
    tmp_i = P.sb("tmp_i", [128, 512], I32); tmp_f = P.sb("tmp_f", [128, 512], F32); dtmp = Dep()
    Ct = P.sb("Ct", [128, TOK], F32); dC = Dep()
    St = P.sb("St", [128, TOK], F32); dS = Dep()
    for sbi in range(NSB):
        sl = slice(sbi * 512, (sbi + 1) * 512)
        P.op("dve", lambda E, sl=sl: E.tensor_copy(out=ang[:], in_=posi[:, sl]), reads=[dposi], writes=[dang])
        P.op("dve", lambda E: E.tensor_scalar(out=ang[:], in0=ang[:], scalar1=rc[:, 0:1], scalar2=None, op0=ALU.mult),
             reads=[dang, drc], writes=[dang])
        emit_sin_table(P, Ct[:, sl], dC, ang, dang, tmp_i, tmp_f, dtmp, np.pi / 2)
        emit_sin_table(P, St[:, sl], dS, ang, dang, tmp_i, tmp_f, dtmp, 0.0)
    P.op("dve", lambda E: E.tensor_scalar(out=St[:], in0=St[:], scalar1=rc[:, 1:2], scalar2=None, op0=ALU.mult),
         reads=[dS, drc], writes=[dS])

    if dbg == 1:
        o_dbg = dram_out(nc, "dbg", [128, TOK], F32)
        dd = Dep()
        P.dma(o_dbg, Ct[:], reads=[dC], writes=[dd])
        o_dbg2 = dram_out(nc, "dbg2", [128, TOK], F32)
        P.dma(o_dbg2, St[:], reads=[dS], writes=[dd])
        return nc, P.finalize(final_deps=[dd])
    ss = P.sb("ss", [128, NT], F32); dss = Dep()
    rstd = P.sb("rstd", [128, NT], F32); drstd = Dep()
    junk = P.sb("junk", [128, D], BF16); djunk = Dep()
    for a in range(NT):
        P.op("act", lambda E, a=a: E.activation(out=junk[:], in_=xt[:, a, :], func=AF.Square, accum_out=ss[:, a:a + 1]),
             reads=[dx], writes=[djunk, dss])
    emit_rms_rstd(P, ss, dss, rstd, drstd, NT)

    hT = P.sb("hT", [128, 8, TOK], BF16); dhT = Dep()
    hb_ring = Ring(P, "hb", 2, [128, D], BF16)
    ptr = P.ps("ptr", [128, 1024], BF16); dptr = Dep()
    for a in range(NT):
        emit_norm_transpose(P, xt, dx, a, rstd, drstd, gbc, dg, hb_ring, ident, dident, ptr, dptr, hT, dhT)

    if dbg == 2:
        o_dbg = dram_out(nc, "dbg", [128, 8, TOK], BF16)
        dd = Dep()
        P.dma(o_dbg, hT[:], reads=[dhT], writes=[dd])
        return nc, P.finalize(final_deps=[dd])
    wst = Ring(P, "wst", 2, [128, 8, 128], F32)
    wbf = Ring(P, "wbf", 3, [128, 8, 128], BF16)
    pacc = Ring(P, "pacc", 4, [128, 512], F32, psum=True)
    obuf = Ring(P, "obuf", 4, [128, 512], BF16)
    t1r = Ring(P, "t1r", 2, [128, 512], F32)
    t2r = Ring(P, "t2r", 2, [128, 512], F32)

    def load_group(gi):
        st, dst = wst.next()
        P.dma(st[:], wfm[gi], writes=[dst])
        wb, dwb = wbf.next()
        P.op(CAST_ENG, lambda E: E.tensor_copy(out=wb[:], in_=st[:]), reads=[dst], writes=[dwb])
        return wb, dwb

    def mm_group(wb, dwb, sbi):
        pt, dpt = pacc.next()
        for kc in range(8):
            P.op("pe", lambda E, kc=kc: E.matmul(pt[:], lhsT=wb[:, kc, :], rhs=hT[:, kc, sbi * 512:(sbi + 1) * 512],
                                                 start=(kc == 0), stop=(kc == 7)), reads=[dwb, dhT], writes=[dpt])
        return pt, dpt

    def store(ob, dob, dst_ap):
        d = Dep()
        P.dma(dst_ap, ob, reads=[dob], writes=[d], prim=dob, eng=STORE_ENG)
        fin.append(dob)

    def plain_group(gi, func, scale, dsts):
        wb, dwb = load_group(gi)
        for sbi in range(NSB):
            pt, dpt = mm_group(wb, dwb, sbi)
            ob, dob = obuf.next()
            P.op("act", lambda E: E.activation(out=ob[:], in_=pt[:], func=func, scale=scale), reads=[dpt], writes=[dob])
            for lo, hi, ap in dsts(sbi):
                store(ob[lo:hi, :], dob, ap)

    def rope_group(gp, gs, scale, dsts_r, dsts_p):
        wp, dwp = load_group(gp)
        ws, dws = load_group(gs)
        for sbi in range(NSB):
            pp, dpp = mm_group(wp, dwp, sbi)
            pq, dpq = mm_group(ws, dws, sbi)
            sl = slice(sbi * 512, (sbi + 1) * 512)
            t1, dt1 = t1r.next()
            t2, dt2 = t2r.next()
            P.op("dve", lambda E: E.tensor_tensor(out=t1[:], in0=pp[:], in1=Ct[:, sl], op=ALU.mult), reads=[dpp, dC], writes=[dt1])
            P.op("dve", lambda E: E.tensor_tensor(out=t2[:], in0=pq[:], in1=St[:, sl], op=ALU.mult), reads=[dpq, dS], writes=[dt2])
            P.op("dve", lambda E: E.tensor_tensor(out=t1[:], in0=t1[:], in1=t2[:], op=ALU.add), reads=[dt1, dt2], writes=[dt1])
            ob, dob = obuf.next()
            P.op("act", lambda E: E.activation(out=ob[:], in_=t1[:], func=AF.Copy, scale=scale), reads=[dt1], writes=[dob])
            for lo, hi, ap in dsts_r(sbi):
                store(ob[lo:hi, :], dob, ap)
            if dsts_p is not None:
                ob2, dob2 = obuf.next()
                P.op("act", lambda E: E.activation(out=ob2[:], in_=pp[:], func=AF.Copy, scale=scale), reads=[dpp], writes=[dob2])
                for lo, hi, ap in dsts_p(sbi):
                    store(ob2[lo:hi, :], dob2, ap)

    def tsl(sbi):
        return slice(sbi * 512, (sbi + 1) * 512)

    for i in range(0 if dbg == 5 else 4):
        rope_group(i, 4 + i, 0.125,
                   lambda sbi, i=i: [(0, 64, o_qr[2 * i, :, tsl(sbi)]), (64, 128, o_qr[2 * i + 1, :, tsl(sbi)])],
                   lambda sbi, i=i: [(0, 64, o_qp[2 * i, :, tsl(sbi)]), (64, 128, o_qp[2 * i + 1, :, tsl(sbi)])])
    if dbg == 3:
        return nc, P.finalize(final_deps=fin)
    plain_group(8, AF.Copy, 1.0, lambda sbi: [(0, 128, o_kcvc[:, tsl(sbi)])])
    if dbg in (4, 5):
        return nc, P.finalize(final_deps=fin)
    rope_group(9, 10, 1.0, lambda sbi: [(0, 128, o_kskw[:, tsl(sbi)])], None)
    for i in range(12):
        plain_group(11 + i, AF.Sigmoid, 1.0,
                    lambda sbi, i=i: [(0, 64, o_g[2 * i, :, tsl(sbi)]), (64, 128, o_g[2 * i + 1, :, tsl(sbi)])])
    for i in range(4):
        plain_group(23 + i, AF.Copy, float(128 ** -0.5), lambda sbi, i=i: [(0, 128, o_sq[i, :, tsl(sbi)])])
    for i in range(4):
        plain_group(27 + i, AF.Copy, 1.0, lambda sbi, i=i: [(0, 128, o_sk[i, :, tsl(sbi)])])
    for i in range(16):
        plain_group(31 + i, AF.Sigmoid, 1.0, lambda sbi, i=i: [(0, 128, o_mg[i, :, tsl(sbi)])])

    wtst = Ring(P, "wtst", 2, [128, 640], F32)
    wtb = P.sb("wtb", [128, 8, 640], BF16); dwtb = Dep()
    for kc in range(8):
        st_, dst_ = wtst.next()
        P.dma(st_[:], wtm[:, kc, :], writes=[dst_])
        P.op("pool", lambda E, kc=kc, st_=st_: E.tensor_copy(out=wtb[:, kc, :], in_=st_[:]), reads=[dst_], writes=[dwtb])
    otm = Ring(P, "otm", 2, [128, 640], BF16)
    for a in range(NT):
        p1, dp1 = pacc.next()
        p2, dp2 = pacc.next()
        for kc in range(8):
            P.op("pe", lambda E, kc=kc: E.matmul(p1[:, 0:128], lhsT=hT[:, kc, a * 128:(a + 1) * 128], rhs=wtb[:, kc, 0:128],
                                                 start=(kc == 0), stop=(kc == 7)), reads=[dwtb, dhT], writes=[dp1])
        for kc in range(8):
            P.op("pe", lambda E, kc=kc: E.matmul(p2[:], lhsT=hT[:, kc, a * 128:(a + 1) * 128], rhs=wtb[:, kc, 128:640],
                                                 start=(kc == 0), stop=(kc == 7)), reads=[dwtb, dhT], writes=[dp2])
        ot, dot = otm.next()
        P.op("act", lambda E: E.activation(out=ot[:, 0:128], in_=p1[:, 0:128], func=AF.Copy), reads=[dp1], writes=[dot])
        P.op("dve", lambda E: E.tensor_copy(out=ot[:, 128:640], in_=p2[:]), reads=[dp2], writes=[dot])
        store(ot[:, 0:128], dot, o_vv[a * 128:(a + 1) * 128, :])
        store(ot[:, 128:640], dot, o_sv[a * 128:(a + 1) * 128, :])

    st = P.finalize(final_deps=fin)
    return nc, st


def layout_c_weights(w_nsa_o, w_sb_o, w_out, w_ff1, w_ff2):
    wn = np.ascontiguousarray(w_nsa_o.reshape(8, 64, D).transpose(1, 0, 2))
    ws = np.ascontiguousarray(w_sb_o.reshape(4, 128, D).transpose(1, 0, 2))
    wo = np.ascontiguousarray(w_out.reshape(8, 128, D).transpose(1, 0, 2))
    w1 = np.ascontiguousarray(w_ff1.reshape(8, 128, DFF).transpose(1, 0, 2))
    w2 = np.ascontiguousarray(w_ff2.reshape(32, 128, D).transpose(1, 0, 2))
    return wn, ws, wo, w1, w2


def build_phase_c():
    nc = bass.Bass("TRN2", target_bir_lowering=False)
    x = dram_in(nc, "x", [TOK, D], F32)
    onsa = dram_in(nc, "onsaT", [8, 64, TOK], BF16)
    osb = dram_in(nc, "osbT", [4, 128, TOK], BF16)
    mg = dram_in(nc, "mgT", [16, 128, TOK], BF16)
    g_in = dram_in(nc, "g123", [3, 128, D], F32)
    ident_in = dram_in(nc, "ident", [128, 128], F32)
    wn_in = dram_in(nc, "wn", [64, 8, D], F32)
    ws_in = dram_in(nc, "ws", [128, 4, D], F32)
    wo_in = dram_in(nc, "wo", [128, 8, D], F32)
    w1_in = dram_in(nc, "w1", [128, 8, DFF], F32)
    w2_in = dram_in(nc, "w2", [128, 32, D], F32)
    xo = dram_out(nc, "xo", [TOK, D], F32)
    P = Prog(nc)
    fin = []
    NT = TOK // 128

    xt = P.sb("xt", [128, NT, D], F32); dx = Dep()
    for a in range(NT):
        P.dma(xt[:, a, :], x[a * 128:(a + 1) * 128, :], writes=[dx])
    g123 = P.sb("g123", [128, 3, D], F32); dg = Dep()
    for i in range(3):
        P.dma(g123[:, i, :], g_in[i], writes=[dg])
    idf = P.sb("idf", [128, 128], F32); didf = Dep()
    P.dma(idf[:], ident_in, writes=[didf])
    ident = P.sb("ident", [128, 128], BF16); dident = Dep()
    P.op("dve", lambda E: E.tensor_copy(out=ident[:], in_=idf[:]), reads=[didf], writes=[dident])
    ss = P.sb("ss", [128, 4], F32); dss = Dep()
    rstd = P.sb("rstd", [128, 4], F32); drstd = Dep()
    junk = P.sb("junk", [128, 512], BF16); djunk = Dep()
    pacc = Ring(P, "pacc", 7, [128, 512], F32, psum=True)
    ptr = P.ps("ptr", [128, 1024], BF16); dptr = Dep()
    stg = Ring(P, "stg", 2, [128, 2048], F32)

    def rms_from_psum(p0, d0, p1, d1):
        P.op("act", lambda E: E.activation(out=junk[:], in_=p0[:], func=AF.Square, accum_out=ss[:, 0:1]), reads=[d0], writes=[djunk, dss])
        P.op("act", lambda E: E.activation(out=junk[:], in_=p1[:], func=AF.Square, accum_out=ss[:, 1:2]), reads=[d1], writes=[djunk, dss])
        P.op("dve", lambda E: E.tensor_tensor(out=ss[:, 2:3], in0=ss[:, 0:1], in1=ss[:, 1:2], op=ALU.add), reads=[dss], writes=[dss])
        P.op("dve", lambda E: E.tensor_scalar(out=rstd[:, 0:1], in0=ss[:, 2:3], scalar1=1.0 / D, scalar2=EPS, op0=ALU.mult, op1=ALU.add), reads=[dss], writes=[drstd])
        P.op("act", lambda E: E.activation(out=rstd[:, 0:1], in_=rstd[:, 0:1], func=AF.Sqrt), reads=[drstd], writes=[drstd])
        P.op("dve", lambda E: E.reciprocal(out=rstd[:, 0:1], in_=rstd[:, 0:1]), reads=[drstd], writes=[drstd])

    P.open_scope()
    wn = P.sb("wn", [64, 8, D], BF16); dwn = Dep()
    ws = P.sb("ws", [128, 4, D], BF16); dws = Dep()
    wo = P.sb("wo", [128, 8, D], BF16); dwo = Dep()
    for h in range(8):
        st, dst = stg.next()
        P.dma(st[0:64, 0:D], wn_in[:, h, :], writes=[dst])
        P.op("pool", lambda E: E.tensor_copy(out=wn[:, h, :], in_=st[0:64, 0:D]), reads=[dst], writes=[dwn])
    for h in range(4):
        st, dst = stg.next()
        P.dma(st[:, 0:D], ws_in[:, h, :], writes=[dst])
        P.op("pool", lambda E: E.tensor_copy(out=ws[:, h, :], in_=st[:, 0:D]), reads=[dst], writes=[dws])
    for h in range(8):
        st, dst = stg.next()
        P.dma(st[:, 0:D], wo_in[:, h, :], writes=[dst])
        P.op("pool", lambda E: E.tensor_copy(out=wo[:, h, :], in_=st[:, 0:D]), reads=[dst], writes=[dwo])
    on = P.sb("on", [64, 8, 512], BF16); don = Dep()
    os_ = P.sb("os", [128, 4, 512], BF16); dos = Dep()
    mgt = P.sb("mgt", [128, 16, 512], BF16); dmg = Dep()
    merged = P.sb("merged", [128, 8, 512], BF16); dmer = Dep()
    t1r = Ring(P, "t1r", 2, [128, 512], F32)
    t2r = Ring(P, "t2r", 2, [128, 512], F32)
    for j in range(NSB):
        sl = slice(j * 512, (j + 1) * 512)
        for h in range(8):
            P.dma(on[:, h, :], onsa[h, :, sl], writes=[don])
        for h in range(4):
            P.dma(os_[:, h, :], osb[h, :, sl], writes=[dos])
        for h in range(16):
            P.dma(mgt[:, h, :], mg[h, :, sl], writes=[dmg])
        for dc in range(8):
            py, dpy = pacc.next()
            pz, dpz = pacc.next()
            for h in range(8):
                P.op("pe", lambda E: E.matmul(py[:], lhsT=wn[:, h, dc * 128:(dc + 1) * 128], rhs=on[:, h, :], start=(h == 0), stop=(h == 7)),
                     reads=[dwn, don], writes=[dpy])
            for h in range(4):
                P.op("pe", lambda E: E.matmul(pz[:], lhsT=ws[:, h, dc * 128:(dc + 1) * 128], rhs=os_[:, h, :], start=(h == 0), stop=(h == 3)),
                     reads=[dws, dos], writes=[dpz])
            t1, dt1 = t1r.next()
            t2, dt2 = t2r.next()
            P.op("dve", lambda E: E.tensor_tensor(out=t1[:], in0=py[:], in1=mgt[:, dc, :], op=ALU.mult), reads=[dpy, dmg], writes=[dt1])
            P.op("dve", lambda E: E.tensor_tensor(out=t2[:], in0=pz[:], in1=mgt[:, 8 + dc, :], op=ALU.mult), reads=[dpz, dmg], writes=[dt2])
            P.op("dve", lambda E: E.tensor_tensor(out=merged[:, dc, :], in0=t1[:], in1=t2[:], op=ALU.add), reads=[dt1, dt2], writes=[dmer])
        for tt in range(4):
            a = 4 * j + tt
            pm = []
            for half in range(2):
                p_, dp_ = pacc.next()
                for dc in range(8):
                    P.op("pe", lambda E: E.matmul(p_[:], lhsT=merged[:, dc, tt * 128:(tt + 1) * 128], rhs=wo[:, dc, half * 512:(half + 1) * 512],
                                                 start=(dc == 0), stop=(dc == 7)), reads=[dmer, dwo], writes=[dp_])
                pm.append((p_, dp_))
            rms_from_psum(pm[0][0], pm[0][1], pm[1][0], pm[1][1])
            for half in range(2):
                t1, dt1 = t1r.next()
                hs = slice(half * 512, (half + 1) * 512)
                P.op("dve", lambda E: E.scalar_tensor_tensor(out=t1[:], in0=pm[half][0][:], scalar=rstd[:, 0:1], in1=g123[:, 0, hs],
                                                             op0=ALU.mult, op1=ALU.mult), reads=[pm[half][1], drstd, dg], writes=[dt1])
                P.op("dve", lambda E: E.tensor_tensor(out=xt[:, a, hs], in0=xt[:, a, hs], in1=t1[:], op=ALU.add), reads=[dx, dt1], writes=[dx])
    P.close_scope()

    P.open_scope()
    hfT = P.sb("hfT", [128, 8, 512], BF16); dhT = Dep()
    h1 = P.sb("h1", [128, 32, 512], BF16); dh1 = Dep()
    hb_ring = Ring(P, "hb", 2, [128, D], BF16)
    w1b = Ring(P, "w1b", 2, [128, 8, 256], BF16)
    w2b = Ring(P, "w2b", 2, [128, D], BF16)
    rl = Ring(P, "rl", 2, [128, 512], F32)
    ss4 = P.sb("ss4", [128, 4], F32); dss4 = Dep()
    rs4 = P.sb("rs4", [128, 4], F32); drs4 = Dep()
    junk2 = P.sb("junk2", [128, D], BF16); djunk2 = Dep()
    t1r = Ring(P, "t1f", 2, [128, 512], F32)
    for j in range(NSB):
        for tt in range(4):
            a = 4 * j + tt
            P.op("act", lambda E: E.activation(out=junk2[:], in_=xt[:, a, :], func=AF.Square, accum_out=ss4[:, tt:tt + 1]),
                 reads=[dx], writes=[djunk2, dss4])
        emit_rms_rstd(P, ss4, dss4, rs4, drs4, 4)
        for tt in range(4):
            a = 4 * j + tt
            hb, dhb = hb_ring.next()
            P.op("dve", lambda E: E.scalar_tensor_tensor(out=hb[:], in0=xt[:, a, :], scalar=rs4[:, tt:tt + 1], in1=g123[:, 1, :],
                                                         op0=ALU.mult, op1=ALU.mult), reads=[dx, drs4, dg], writes=[dhb])
            for kc in range(8):
                P.op("pe", lambda E: E.transpose(ptr[:, kc * 128:(kc + 1) * 128], hb[:, kc * 128:(kc + 1) * 128], ident[:]),
                     reads=[dhb, dident], writes=[dptr])
            P.op("act", lambda E: E.activation(out=hfT[:, :, tt * 128:(tt + 1) * 128], in_=ptr[:].rearrange("p (k t) -> p k t", k=8),
                                               func=AF.Copy), reads=[dptr], writes=[dhT])
        for fg in range(16):
            st, dst = stg.next()
            P.dma(st[:].rearrange("p (k c) -> p k c", k=8), w1_in[:, :, fg * 256:(fg + 1) * 256], writes=[dst])
            wb, dwb = w1b.next()
            P.op("pool", lambda E: E.tensor_copy(out=wb[:], in_=st[:].rearrange("p (k c) -> p k c", k=8)), reads=[dst], writes=[dwb])
            for f2 in range(2):
                fc = fg * 2 + f2
                ph, dph = pacc.next()
                for kc in range(8):
                    P.op("pe", lambda E: E.matmul(ph[:], lhsT=wb[:, kc, f2 * 128:(f2 + 1) * 128], rhs=hfT[:, kc, :], start=(kc == 0), stop=(kc == 7)),
                         reads=[dwb, dhT], writes=[dph])
                r_, dr_ = rl.next()
                P.op("act", lambda E: E.activation(out=r_[:], in_=ph[:], func=AF.Relu), reads=[dph], writes=[dr_])
                P.op("dve", lambda E: E.tensor_tensor(out=h1[:, fc, :], in0=r_[:], in1=r_[:], op=ALU.mult), reads=[dr_], writes=[dh1])
        for tp in range(2):
            accs = [pacc.next() for _ in range(4)]
            for fc in range(32):
                st, dst = stg.next()
                P.dma(st[:, 0:D], w2_in[:, fc, :], writes=[dst])
                wb, dwb = w2b.next()
                P.op("pool", lambda E: E.tensor_copy(out=wb[:], in_=st[:, 0:D]), reads=[dst], writes=[dwb])
                for t2 in range(2):
                    tt = tp * 2 + t2
                    for half in range(2):
                        p_, dp_ = accs[t2 * 2 + half]
                        P.op("pe", lambda E: E.matmul(p_[:], lhsT=h1[:, fc, tt * 128:(tt + 1) * 128], rhs=wb[:, half * 512:(half + 1) * 512],
                                                     start=(fc == 0), stop=(fc == 31)), reads=[dh1, dwb], writes=[dp_])
            for t2 in range(2):
                tt = tp * 2 + t2
                a = 4 * j + tt
                (p0, d0), (p1, d1) = accs[t2 * 2], accs[t2 * 2 + 1]
                rms_from_psum(p0, d0, p1, d1)
                for half, (p_, dp_) in enumerate(((p0, d0), (p1, d1))):
                    t1, dt1 = t1r.next()
                    hs = slice(half * 512, (half + 1) * 512)
                    P.op("dve", lambda E: E.scalar_tensor_tensor(out=t1[:], in0=p_[:], scalar=rstd[:, 0:1], in1=g123[:, 2, hs],
                                                                 op0=ALU.mult, op1=ALU.mult), reads=[dp_, drstd, dg], writes=[dt1])
                    P.op("dve", lambda E: E.tensor_tensor(out=xt[:, a, hs], in0=xt[:, a, hs], in1=t1[:], op=ALU.add), reads=[dx, dt1], writes=[dx])
                dd = Dep()
                P.dma(xo[a * 128:(a + 1) * 128, :], xt[:, a, :], reads=[dx], writes=[dd])
                fin.append(dd)
    P.close_scope()
    st = P.finalize(final_deps=fin)
    return nc, st


def sb_mask_table(parity):
    p = np.arange(128)[:, None, None]
    i = np.arange(8)[None, :, None]
    f = np.arange(512)[None, None, :]
    return (512 * parity + f - 128 * i - p > 0)


def build_phase_sb(n_m=16):
    nc = bass.Bass("TRN2", target_bir_lowering=False)
    sq_in = dram_in(nc, "sq", [128, 16, 512], BF16)
    sk_in = dram_in(nc, "sk", [128, S], BF16)
    sv_in = dram_in(nc, "sv", [128, 128, 128], BF16)
    mk_in = dram_in(nc, "mk", [128, 8, 512], BF16)
    tri_in = dram_in(nc, "tri", [128, 128], BF16)
    ones_in = dram_in(nc, "ones", [128, 128], BF16)
    out = dram_out(nc, "osb", [128, 16, 512], BF16)
    P = Prog(nc)
    fin = []
    sq = P.sb("sq", [128, 16, 512], BF16); dsq = Dep()
    P.dma(sq[:], sq_in, writes=[dsq])
    sk = P.sb("sk", [128, S], BF16); dsk = Dep()
    nsk = P.sb("nsk", [128, S], BF16); dnsk = Dep()
    for i in range(4):
        sl = slice(i * 4096, (i + 1) * 4096)
        P.dma(sk[:, sl], sk_in[:, sl], writes=[dsk])
    for i in range(4):
        sl = slice(i * 4096, (i + 1) * 4096)
        P.op("pool", lambda E: E.tensor_scalar(out=nsk[:, sl], in0=sk[:, sl], scalar1=-1.0, scalar2=None, op0=ALU.mult), reads=[dsk], writes=[dnsk])
    sv = P.sb("sv", [128, 128, 128], BF16); dsv = Dep()
    for i in range(4):
        P.dma(sv[:, i * 32:(i + 1) * 32, :], sv_in[:, i * 32:(i + 1) * 32, :], writes=[dsv])
    mk = P.sb("mk", [128, 8, 512], BF16); dmk = Dep()
    P.dma(mk[:], mk_in, writes=[dmk])
    tri = P.sb("tri", [128, 128], BF16); dtri = Dep()
    P.dma(tri[:], tri_in, writes=[dtri])
    ones = P.sb("ones", [128, 128], BF16); dones = Dep()
    P.dma(ones[:], ones_in, writes=[dones])
    pzr = Ring(P, "pz", 2, [128, 512], F32, psum=True)
    pcr = Ring(P, "pc", 2, [128, 512], F32, psum=True)
    por = Ring(P, "po", 2, [128, 512], F32, psum=True)
    er = Ring(P, "e", 2, [128, 512], F32)
    spr = Ring(P, "sp", 3, [128, 512], BF16)
    ar = Ring(P, "a", 3, [128, 512], BF16)
    R = P.sb("R", [128, 512], F32); dR = Dep()
    rbr = Ring(P, "rb", 2, [128, 512], BF16)
    obr = Ring(P, "ob", 2, [128, 512], BF16)
    for m in range(n_m):
        L = 8 * (m + 1)
        po, dpo = por.next()
        rb, drb = None, None
        for idx, kc in enumerate(reversed(range(L))):
            ti = kc - (L - 8)
            first, last = idx == 0, idx == L - 1
            ks = slice(kc * 128, (kc + 1) * 128)
            pz, dpz = pzr.next()
            P.op("pe", lambda E: E.matmul(pz[:], lhsT=sk[:, ks], rhs=sq[:, m, :], start=True, stop=True), reads=[dsk, dsq], writes=[dpz])
            e, de = er.next()
            P.op("act", lambda E: E.activation(out=e[:], in_=pz[:], func=AF.Exp), reads=[dpz], writes=[de])
            sp, dsp = spr.next()
            P.op("act", lambda E: E.activation(out=sp[:], in_=e[:], func=AF.Ln, bias=1.0), reads=[de], writes=[dsp])
            if ti >= 0:
                P.op("dve", lambda E: E.tensor_tensor(out=sp[:], in0=sp[:], in1=mk[:, ti, :], op=ALU.mult), reads=[dsp, dmk], writes=[dsp])
            pc, dpc = pcr.next()
            P.op("pe", lambda E: E.matmul(pc[:], lhsT=tri[:], rhs=sp[:], start=True, stop=False), reads=[dtri, dsp], writes=[dpc])
            if not first:
                P.op("pe", lambda E: E.matmul(pc[:], lhsT=ones[:], rhs=rb[:], start=False, stop=False), reads=[dones, drb], writes=[dpc])
            P.op("pe", lambda E: E.matmul(pc[:], lhsT=nsk[:, ks], rhs=sq[:, m, :], start=False, stop=True), reads=[dnsk, dsq], writes=[dpc])
            a, da = ar.next()
            P.op("act", lambda E: E.activation(out=a[:], in_=pc[:], func=AF.Exp, scale=-1.0), reads=[dpc], writes=[da])
            if ti >= 0:
                P.op("dve", lambda E: E.tensor_tensor(out=a[:], in0=a[:], in1=mk[:, ti, :], op=ALU.mult), reads=[da, dmk], writes=[da])
            P.op("pe", lambda E: E.matmul(po[:], lhsT=sv[:, kc, :], rhs=a[:], start=first, stop=last), reads=[dsv, da], writes=[dpo])
            if not last:
                if first:
                    P.op("dve", lambda E: E.tensor_copy(out=R[:], in_=sp[:]), reads=[dsp], writes=[dR])
                else:
                    P.op("dve", lambda E: E.tensor_tensor(out=R[:], in0=R[:], in1=sp[:], op=ALU.add), reads=[dR, dsp], writes=[dR])
                rb, drb = rbr.next()
                P.op("pool", lambda E: E.tensor_copy(out=rb[:], in_=R[:]), reads=[dR], writes=[drb])
        ob, dob = obr.next()
        P.op("act", lambda E: E.activation(out=ob[:], in_=po[:], func=AF.Copy), reads=[dpo], writes=[dob])
        dd = Dep()
        P.dma(out[:, m, :], ob[:], reads=[dob], writes=[dd], prim=dob)
        fin.append(dob)
    st = P.finalize(final_deps=fin)
    return nc, st


def etab_const():
    key = np.arange(S)
    r = np.arange(64)[:, None]
    return ((key[None, :] // 64) % 64 == r)


def build_phase_b2(n_g=32):
    nc = bass.Bass("TRN2", target_bir_lowering=False)
    qr_in = dram_in(nc, "qr", [64, S], BF16)
    ks_in = dram_in(nc, "ks", [64, S], BF16)
    kw_in = dram_in(nc, "kw", [64, S], BF16)
    vs_in = dram_in(nc, "vs", [128, 128, 64], BF16)
    vw_in = dram_in(nc, "vw", [128, 128, 64], BF16)
    nm_in = dram_in(nc, "negMT", [256, S], BF16)
    oc_in = dram_in(nc, "oc", [64, S], BF16)
    g_in = dram_in(nc, "G3", [3, 64, S], BF16)
    et_in = dram_in(nc, "etab", [64, S], BF16)
    out = dram_out(nc, "onsa", [64, S], BF16)
    P = Prog(nc)
    fin = []
    KE = P.sb("KE", [128, S], BF16); dKE = Dep()
    KW = P.sb("KW", [64, S], BF16); dKW = Dep()
    for i in range(4):
        sl = slice(i * 4096, (i + 1) * 4096)
        P.dma(KE[0:64, sl], ks_in[:, sl], writes=[dKE])
        P.dma(KE[64:128, sl], et_in[:, sl], writes=[dKE])
        P.dma(KW[:, sl], kw_in[:, sl], writes=[dKW])
    VS = P.sb("VS", [128, 128, 128], BF16); dVS = Dep()
    VW = P.sb("VW", [128, 128, 128], BF16); dVW = Dep()
    P.op("pool", lambda E: E.memset(VS[:, :, 64:128], 1.0), writes=[dVS])
    P.op("pool", lambda E: E.memset(VW[:, :, 64:128], 1.0), writes=[dVW])
    for i in range(4):
        cs = slice(i * 32, (i + 1) * 32)
        P.dma(VS[:, cs, 0:64], vs_in[:, cs, :], writes=[dVS])
        P.dma(VW[:, cs, 0:64], vw_in[:, cs, :], writes=[dVW])
    qmr = Ring(P, "qm", 8, [128, 512], BF16)
    psr = Ring(P, "ps", 3, [128, 512], F32, psum=True)
    posr = Ring(P, "pos", 2, [128, 512], F32, psum=True)
    powr = Ring(P, "pow", 2, [128, 512], F32, psum=True)
    pr = Ring(P, "p", 4, [128, 512], BF16)
    gr = Ring(P, "g", 2, [64, 3, 512], BF16)
    ocr = Ring(P, "oc", 2, [64, 512], BF16)
    rr = Ring(P, "r", 2, [64, 512], F32)
    tsr = Ring(P, "ts", 2, [64, 512], F32)
    twr = Ring(P, "tw", 2, [64, 512], F32)
    obr = Ring(P, "ob", 2, [64, 512], BF16)

    def attend(pacc, dpacc, chunks, lhs_fn, rhs_fn, V, dV, g, win):
        n = len(chunks)
        for i, kc in enumerate(chunks):
            ps, dps = psr.next()
            lhsT, dl = lhs_fn(kc)
            rhs, drh = rhs_fn(kc)
            P.op("pe", lambda E: E.matmul(ps[:], lhsT=lhsT, rhs=rhs, start=True, stop=True), reads=[dl, drh], writes=[dps])
            p, dp = pr.next()
            P.op("act", lambda E: E.activation(out=p[:], in_=ps[:], func=AF.Exp), reads=[dps], writes=[dp])
            if kc >= 4 * g:
                P.op("pool", lambda E: E.affine_select(out=p[:], in_=p[:], pattern=[[1, 512]], compare_op=ALU.is_ge, fill=0.0,
                                                       base=-128 * (kc - 4 * g), channel_multiplier=-1), reads=[dp], writes=[dp])
            elif win:
                P.op("pool", lambda E: E.affine_select(out=p[:], in_=p[:], pattern=[[-1, 512]], compare_op=ALU.is_ge, fill=0.0,
                                                       base=128 * (kc - 4 * g) + 511, channel_multiplier=1), reads=[dp], writes=[dp])
            P.op("pe", lambda E: E.matmul(pacc[:], lhsT=V[:, kc, :], rhs=p[:], start=(i == 0), stop=(i == n - 1)), reads=[dV, dp], writes=[dpacc])

    for g in range(n_g):
        sl = slice(g * 512, (g + 1) * 512)
        nch = 4 * g + 4
        ngrp = (nch - 1) // 32 + 1
        qms = []
        for gi in range(ngrp):
            qm, dqm = qmr.next()
            P.dma(qm[0:64, :], qr_in[:, sl], writes=[dqm])
            P.dma(qm[64:128, :], nm_in[64 * gi:64 * gi + 64, sl], writes=[dqm])
            qms.append((qm, dqm))
        gt, dgt = gr.next()
        for b in range(3):
            P.dma(gt[:, b, :], g_in[b, :, sl], writes=[dgt])
        oct_, doc = ocr.next()
        P.dma(oct_[:], oc_in[:, sl], writes=[doc])
        pos_, dpos = posr.next()
        attend(pos_, dpos, list(range(nch)),
               lambda kc: (KE[:, kc * 128:(kc + 1) * 128], dKE),
               lambda kc: (qms[kc // 32][0][:], qms[kc // 32][1]), VS, dVS, g, False)
        pow_, dpow = powr.next()
        attend(pow_, dpow, list(range(max(0, 4 * g - 4), nch)),
               lambda kc: (KW[0:64, kc * 128:(kc + 1) * 128], dKW),
               lambda kc: (qms[0][0][0:64, :], qms[0][1]), VW, dVW, g, True)
        r1, dr1 = rr.next()
        ts, dts = tsr.next()
        P.op("dve", lambda E: E.reciprocal(out=r1[:], in_=pos_[64:128, :]), reads=[dpos], writes=[dr1])
        P.op("dve", lambda E: E.tensor_tensor(out=ts[:], in0=pos_[0:64, :], in1=r1[:], op=ALU.mult), reads=[dpos, dr1], writes=[dts])
        P.op("dve", lambda E: E.tensor_tensor(out=ts[:], in0=ts[:], in1=gt[:, 1, :], op=ALU.mult), reads=[dts, dgt], writes=[dts])
        r2, dr2 = rr.next()
        tw, dtw = twr.next()
        P.op("dve", lambda E: E.reciprocal(out=r2[:], in_=pow_[64:128, :]), reads=[dpow], writes=[dr2])
        P.op("dve", lambda E: E.tensor_tensor(out=tw[:], in0=pow_[0:64, :], in1=r2[:], op=ALU.mult), reads=[dpow, dr2], writes=[dtw])
        P.op("dve", lambda E: E.tensor_tensor(out=tw[:], in0=tw[:], in1=gt[:, 2, :], op=ALU.mult), reads=[dtw, dgt], writes=[dtw])
        P.op("dve", lambda E: E.tensor_tensor(out=ts[:], in0=ts[:], in1=tw[:], op=ALU.add), reads=[dts, dtw], writes=[dts])
        P.op("dve", lambda E: E.tensor_tensor(out=tw[:], in0=oct_[:], in1=gt[:, 0, :], op=ALU.mult), reads=[doc, dgt], writes=[dtw])
        ob, dob = obr.next()
        P.op("dve", lambda E: E.tensor_tensor(out=ob[:], in0=ts[:], in1=tw[:], op=ALU.add), reads=[dts, dtw], writes=[dob])
        dd = Dep()
        P.dma(out[:, sl], ob[:], reads=[dob], writes=[dd], prim=dob)
        fin.append(dob)
    st = P.finalize(final_deps=fin)
    return nc, st


def b1_consts(core):
    c = np.arange(1024)
    blk = np.arange(256)
    c_start = 16 * c[:, None]
    s_start = 64 * blk[None, :]
    ov = ((c_start < s_start + 64) & (c_start + 32 > s_start)).astype(np.float32)
    ov[1023, :] = 0.0
    ovl = np.concatenate([ov, np.ones((1024, 1), np.float32)], 1).reshape(8, 128, 257).transpose(1, 0, 2)
    t = core * TOK + np.arange(TOK)
    tq = np.broadcast_to(t[None, :].astype(np.float32), (128, TOK))
    cval = (16 * (128 * np.arange(8)[None, :] + np.arange(128)[:, None]) + 31).astype(np.float32)
    bi = np.broadcast_to(blk[None, :].astype(np.float32), (128, 256)).copy()
    bic = bi.copy(); bic[:, 0] = 1e9
    b0 = np.zeros((128, 256), np.float32); b0[:, 0] = 1.0
    cur = (t // 64).reshape(TOK // 128, 128).T.astype(np.float32)
    curt = np.stack([cur, cur - 1, cur - 2], 2)
    return dict(ovl=np.ascontiguousarray(ovl), tq=np.ascontiguousarray(tq), cval=cval, bi=bi, bic=bic, b0=b0,
                curt=np.ascontiguousarray(curt))


def layout_cmp_weights(cmp_pe, cmp_w1, cmp_w2):
    w1 = np.ascontiguousarray(cmp_w1.reshape(2, 16, 128, 256).transpose(0, 2, 1, 3))
    w2 = np.ascontiguousarray(cmp_w2.reshape(2, 2, 128, 64).transpose(0, 2, 1, 3))
    pef = np.ascontiguousarray(cmp_pe.reshape(2, 16, 2, 64).transpose(0, 2, 3, 1).reshape(2, 128, 16))
    return w1, w2, pef


def build_phase_b1():
    nc = bass.Bass("TRN2", target_bir_lowering=False)
    kv_in = dram_in(nc, "kcvc", [128, S + 16], BF16)
    qp_in = dram_in(nc, "qp", [8, 64, TOK], BF16)
    w1_in = dram_in(nc, "cw1", [2, 128, 16, 256], F32)
    w2_in = dram_in(nc, "cw2", [2, 128, 2, 64], F32)
    pe_in = dram_in(nc, "pef", [2, 128, 16], F32)
    ovl_in = dram_in(nc, "ovl", [128, 8, 257], F32)
    tq_in = dram_in(nc, "tq", [128, TOK], F32)
    cval_in = dram_in(nc, "cval", [128, 8], F32)
    bi_in = dram_in(nc, "bi", [128, 256], F32)
    bic_in = dram_in(nc, "bic", [128, 256], F32)
    b0_in = dram_in(nc, "b0", [128, 256], F32)
    curt_in = dram_in(nc, "curt", [128, 16, 3], F32)
    ident_in = dram_in(nc, "ident", [128, 128], F32)
    o_oc = dram_out(nc, "ocT", [8, 64, TOK], BF16)
    o_nm = dram_out(nc, "negMT", [256, TOK], BF16)
    P = Prog(nc)
    fin = []
    NT = TOK // 128

    def load_const(name, src, shape, dt=F32):
        t = P.sb(name, shape, dt); d = Dep()
        P.dma(t[:], src, writes=[d])
        return t, d
    tq, dtq = load_const("tq", tq_in, [128, TOK])
    cval, dcv = load_const("cval", cval_in, [128, 8])
    bi, dbi = load_const("bi", bi_in, [128, 256])
    bic, dbic = load_const("bic", bic_in, [128, 256])
    b0, db0 = load_const("b0", b0_in, [128, 256])
    curt, dcur = load_const("curt", curt_in, [128, 16, 3])
    idf, didf = load_const("idf", ident_in, [128, 128])
    ident = P.sb("ident", [128, 128], BF16); dident = Dep()
    P.op("dve", lambda E: E.tensor_copy(out=ident[:], in_=idf[:]), reads=[didf], writes=[dident])
    ovf, dovf = load_const("ovf", ovl_in, [128, 8, 257])
    ovl = P.sb("ovl", [128, 8, 257], BF16); dovl = Dep()
    P.op("dve", lambda E: E.tensor_copy(out=ovl[:], in_=ovf[:]), reads=[dovf], writes=[dovl])

    pacc = Ring(P, "pacc", 4, [128, 512], F32, psum=True)
    por = Ring(P, "po", 2, [128, 512], F32, psum=True)
    ptr = P.ps("ptr", [128, 256], BF16); dptr = Dep()
    kcT = P.sb("kcT", [64, 1024], BF16); dkcT = Dep()
    vca = P.sb("vca", [128, 8, 128], BF16); dvca = Dep()
    P.op("pool", lambda E: E.memset(vca[:], 1.0), writes=[dvca])

    P.open_scope()
    Dk = P.sb("Dk", [128, S], BF16); dDk = Dep()
    hid = P.sb("hid", [128, 2, 1024], BF16); dhid = Dep()
    w1f = P.sb("w1f", [128, 16, 256], F32); dw1f = Dep()
    w1b = P.sb("w1b", [128, 16, 256], BF16); dw1b = Dep()
    w2f = P.sb("w2f", [128, 2, 64], F32); dw2f = Dep()
    w2b = P.sb("w2b", [128, 2, 64], BF16); dw2b = Dep()
    pf = P.sb("pf", [128, 16], F32); dpf = Dep()
    pfb = P.sb("pfb", [128, 16], BF16); dpfb = Dep()
    bias = P.sb("bias", [128, 2], F32); dbias = Dep()
    Dv = Dk[:].rearrange("p (c s) -> p c s", s=16)
    for kv in range(2):
        rb = 64 * kv
        for i in range(4):
            sl = slice(i * 4096, (i + 1) * 4096)
            P.dma(Dk[0:64, sl], kv_in[rb:rb + 64, i * 4096:(i + 1) * 4096], writes=[dDk])
            P.dma(Dk[64:128, sl], kv_in[rb:rb + 64, i * 4096 + 1:(i + 1) * 4096 + 1], writes=[dDk])
        P.dma(w1f[:], w1_in[kv], writes=[dw1f])
        P.op("pool", lambda E: E.tensor_copy(out=w1b[:], in_=w1f[:]), reads=[dw1f], writes=[dw1b])
        P.dma(w2f[:], w2_in[kv], writes=[dw2f])
        P.op("dve", lambda E: E.tensor_copy(out=w2b[:], in_=w2f[:]), reads=[dw2f], writes=[dw2b])
        P.dma(pf[:], pe_in[kv], writes=[dpf])
        P.op("dve", lambda E: E.tensor_copy(out=pfb[:], in_=pf[:]), reads=[dpf], writes=[dpfb])
        P.op("dve", lambda E: E.memset(hid[:], 0.0), writes=[dhid])
        pb, dpb = pacc.next()
        for mc in range(2):
            for l2 in range(16):
                P.op("pe", lambda E: E.matmul(pb[:, mc:mc + 1], lhsT=w1b[:, l2, mc * 128:(mc + 1) * 128], rhs=pfb[:, l2:l2 + 1],
                                             start=(l2 == 0), stop=(l2 == 15)), reads=[dw1b, dpfb], writes=[dpb])
        P.op("dve", lambda E: E.tensor_copy(out=bias[:], in_=pb[:, 0:2]), reads=[dpb], writes=[dbias])
        for mc in range(2):
            for half in range(2):
                n = 512 if half == 0 else 511
                ph, dph = pacc.next()
                for l2 in range(16):
                    P.op("pe", lambda E: E.matmul(ph[:, 0:n], lhsT=w1b[:, l2, mc * 128:(mc + 1) * 128],
                                                 rhs=Dv[:, half * 512 + l2 // 8:half * 512 + l2 // 8 + n, 2 * (l2 % 8)], start=(l2 == 0), stop=(l2 == 15)),
                         reads=[dw1b, dDk], writes=[dph])
                P.op("act", lambda E: E.activation(out=hid[:, mc, half * 512:half * 512 + n], in_=ph[:, 0:n], func=AF.Gelu_apprx_tanh,
                                                   bias=bias[:, mc:mc + 1]), reads=[dph, dbias], writes=[dhid])
        if kv == 0:
            for half in range(2):
                pk, dpk = pacc.next()
                for mc in range(2):
                    P.op("pe", lambda E: E.matmul(pk[0:64, :], lhsT=w2b[:, mc, :], rhs=hid[:, mc, half * 512:(half + 1) * 512],
                                                 start=(mc == 0), stop=(mc == 1)), reads=[dw2b, dhid], writes=[dpk])
                P.op("act", lambda E: E.activation(out=kcT[:, half * 512:(half + 1) * 512], in_=pk[0:64, :], func=AF.Copy), reads=[dpk], writes=[dkcT])
        else:
            for cc in range(8):
                pv, dpv = pacc.next()
                for mc in range(2):
                    P.op("pe", lambda E: E.matmul(pv[:, 0:64], lhsT=hid[:, mc, cc * 128:(cc + 1) * 128], rhs=w2b[:, mc, :],
                                                 start=(mc == 0), stop=(mc == 1)), reads=[dw2b, dhid], writes=[dpv])
                P.op("act", lambda E: E.activation(out=vca[:, cc, 0:64], in_=pv[:, 0:64], func=AF.Copy), reads=[dpv], writes=[dvca])
    P.close_scope()

    impacc = P.sb("impacc", [128, NT, 256], F32); dimp = Dep()
    EM = P.sb("EM", [128, 8, 512], BF16); dEM = Dep()
    qtr = Ring(P, "qt", 2, [64, 512], BF16)
    er = Ring(P, "e", 2, [128, 512], F32)
    rrr = Ring(P, "rr", 2, [64, 512], F32)
    obr = Ring(P, "ob", 2, [64, 512], BF16)
    rinv = Ring(P, "rinv", 2, [128, 1], F32)
    for j in range(NSB):
        sl = slice(j * 512, (j + 1) * 512)
        for h in range(8):
            qt, dqt = qtr.next()
            P.dma(qt[:], qp_in[h, :, sl], writes=[dqt])
            po, dpo = por.next()
            for cc in range(8):
                ps, dps = pacc.next()
                P.op("pe", lambda E: E.matmul(ps[:], lhsT=kcT[:, cc * 128:(cc + 1) * 128], rhs=qt[:], start=True, stop=True), reads=[dkcT, dqt], writes=[dps])
                e, de = er.next()
                P.op("act", lambda E: E.activation(out=e[:], in_=ps[:], func=AF.Exp), reads=[dps], writes=[de])
                P.op("dve", lambda E: E.scalar_tensor_tensor(out=EM[:, cc, :], in0=tq[:, sl], scalar=cval[:, cc:cc + 1], in1=e[:],
                                                             op0=ALU.is_ge, op1=ALU.mult), reads=[dtq, dcv, de], writes=[dEM])
                P.op("pe", lambda E: E.matmul(po[:], lhsT=vca[:, cc, :], rhs=EM[:, cc, :], start=(cc == 0), stop=(cc == 7)), reads=[dvca, dEM], writes=[dpo])
            r_, dr_ = rrr.next()
            P.op("dve", lambda E: E.tensor_scalar(out=r_[:], in0=po[64:128, :], scalar1=1e-30, scalar2=None, op0=ALU.max), reads=[dpo], writes=[dr_])
            P.op("dve", lambda E: E.reciprocal(out=r_[:], in_=r_[:]), reads=[dr_], writes=[dr_])
            ob, dob = obr.next()
            P.op("dve", lambda E: E.tensor_tensor(out=ob[:], in0=po[0:64, :], in1=r_[:], op=ALU.mult), reads=[dpo, dr_], writes=[dob])
            dd = Dep()
            P.dma(o_oc[h, :, sl], ob[:], reads=[dob], writes=[dd], prim=dob)
            fin.append(dob)
            for q4 in range(4):
                a = 4 * j + q4
                pi, dpi = pacc.next()
                for cc in range(8):
                    P.op("pe", lambda E: E.matmul(pi[:, 0:257], lhsT=EM[:, cc, q4 * 128:(q4 + 1) * 128], rhs=ovl[:, cc, :], start=(cc == 0), stop=(cc == 7)),
                         reads=[dEM, dovl], writes=[dpi])
                ri, dri = rinv.next()
                P.op("dve", lambda E: E.tensor_scalar(out=ri[:], in0=pi[:, 256:257], scalar1=1e-30, scalar2=None, op0=ALU.max), reads=[dpi], writes=[dri])
                P.op("dve", lambda E: E.reciprocal(out=ri[:], in_=ri[:]), reads=[dri], writes=[dri])
                if h == 0:
                    P.op("dve", lambda E: E.tensor_scalar(out=impacc[:, a, :], in0=pi[:, 0:256], scalar1=ri[:, 0:1], scalar2=None, op0=ALU.mult),
                         reads=[dpi, dri], writes=[dimp])
                else:
                    P.op("dve", lambda E: E.scalar_tensor_tensor(out=impacc[:, a, :], in0=pi[:, 0:256], scalar=ri[:, 0:1], in1=impacc[:, a, :],
                                                                 op0=ALU.mult, op1=ALU.add), reads=[dpi, dri, dimp], writes=[dimp])
    m1 = P.sb("m1", [128, 256], F32); dm1 = Dep()
    sc = P.sb("sc", [128, 256], F32); dsc = Dep()
    v8 = P.sb("v8", [128, 8], F32); dv8 = Dep()
    Mt = P.sb("Mt", [128, 256], F32); dMt = Dep()
    nmr = Ring(P, "nm", 2, [128, 256], BF16)
    nmtr = Ring(P, "nmt", 2, [128, 2, 128], BF16)
    nm_v = o_nm.rearrange("(b p) t -> p b t", p=128)
    for a in range(NT):
        P.op("dve", lambda E: E.tensor_scalar(out=m1[:], in0=bic[:], scalar1=curt[:, a, 2:3], scalar2=None, op0=ALU.is_le), reads=[dbic, dcur], writes=[dm1])
        P.op("dve", lambda E: E.scalar_tensor_tensor(out=sc[:], in0=impacc[:, a, :], scalar=1.0, in1=m1[:], op0=ALU.add, op1=ALU.mult),
             reads=[dimp, dm1], writes=[dsc])
        P.op("dve", lambda E: E.max(out=v8[:], in_=sc[:]), reads=[dsc], writes=[dv8])
        P.op("dve", lambda E: E.tensor_scalar(out=v8[:, 4:5], in0=v8[:, 4:5], scalar1=0.5, scalar2=None, op0=ALU.max), reads=[dv8], writes=[dv8])
        P.op("dve", lambda E: E.tensor_scalar(out=Mt[:], in0=sc[:], scalar1=v8[:, 4:5], scalar2=None, op0=ALU.is_ge), reads=[dsc, dv8], writes=[dMt])
        P.op("dve", lambda E: E.scalar_tensor_tensor(out=Mt[:], in0=bi[:], scalar=curt[:, a, 0:1], in1=Mt[:], op0=ALU.is_equal, op1=ALU.add),
             reads=[dbi, dcur, dMt], writes=[dMt])
        P.op("dve", lambda E: E.scalar_tensor_tensor(out=Mt[:], in0=bi[:], scalar=curt[:, a, 1:2], in1=Mt[:], op0=ALU.is_equal, op1=ALU.add),
             reads=[dbi, dcur, dMt], writes=[dMt])
        P.op("dve", lambda E: E.tensor_tensor(out=Mt[:], in0=Mt[:], in1=b0[:], op=ALU.add), reads=[dMt, db0], writes=[dMt])
        P.op("dve", lambda E: E.tensor_scalar(out=Mt[:], in0=Mt[:], scalar1=1.0, scalar2=1.0, op0=ALU.min, op1=ALU.subtract), reads=[dMt], writes=[dMt])
        nm, dnm = nmr.next()
        P.op("dve", lambda E: E.tensor_scalar(out=nm[:], in0=Mt[:], scalar1=-NEG, scalar2=None, op0=ALU.mult), reads=[dMt], writes=[dnm])
        for b in range(2):
            P.op("pe", lambda E: E.transpose(ptr[:, b * 128:(b + 1) * 128], nm[:, b * 128:(b + 1) * 128], ident[:]), reads=[dnm, dident], writes=[dptr])
        nmt, dnmt = nmtr.next()
        P.op("act", lambda E: E.activation(out=nmt[:], in_=ptr[:].rearrange("p (b t) -> p b t", b=2), func=AF.Copy), reads=[dptr], writes=[dnmt])
        dd = Dep()
        P.dma(nm_v[:, :, a * 128:(a + 1) * 128], nmt[:], reads=[dnmt], writes=[dd], prim=dnmt)
        fin.append(dnmt)
    st = P.finalize(final_deps=fin)
    return nc, st


_PROGS = {}


def _prog(name, builder):
    nc, _ = builder()
    return nc


def _run(nc, maps):
    res = run_bass_kernel_spmd(nc, maps, core_ids=list(range(NCORES)))
    return res.results


def _bc(v, n=128):
    return np.ascontiguousarray(np.broadcast_to(np.asarray(v)[None, :], (n, v.shape[0])))


def kernel(x, positions, norm_g, w_in, cmp_pe, cmp_w1, cmp_w2, w_nsa_o, w_sb_o, w_out, w_ff1, w_ff2):
    import ml_dtypes
    bf = ml_dtypes.bfloat16
    x = np.asarray(x, np.float32)
    positions = np.asarray(positions)
    xs = [np.ascontiguousarray(x[0, c * TOK:(c + 1) * TOK]) for c in range(NCORES)]
    pos = [np.ascontiguousarray(np.broadcast_to(positions[0, c * TOK:(c + 1) * TOK][None, :].astype(np.int32), (128, TOK)))
           for c in range(NCORES)]
    ident = np.eye(128, dtype=np.float32)
    rc = rope_consts()
    etab = etab_const().astype(bf)
    tri = np.tril(np.ones((128, 128), np.float32)).astype(bf)
    ones = np.ones((128, 128), bf)
    mks = [sb_mask_table(p).astype(bf) for p in range(2)]
    b1c = [b1_consts(c) for c in range(NCORES)]
    cat = lambda key, rs, ax: np.concatenate([np.asarray(r[key]) for r in rs], axis=ax)
    for l in range(DEPTH):
        ng = np.asarray(norm_g[l], np.float32)
        fm, tm = layout_w_in(np.asarray(w_in[l], np.float32))
        gbc = _bc(ng[0])
        ra = _run(_prog("a", build_phase_a),
                  [dict(x=xs[c], pos=pos[c], gbc=gbc, rc=rc, ident=ident, wfm=fm, wtm=tm) for c in range(NCORES)])
        del fm, tm
        kcvc = np.zeros((128, S + 16), bf)
        kcvc[:, :S] = cat("kcvcT", ra, 1)
        w1, w2, pef = layout_cmp_weights(np.asarray(cmp_pe[l], np.float32), np.asarray(cmp_w1[l], np.float32),
                                         np.asarray(cmp_w2[l], np.float32))
        maps = []
        for c in range(NCORES):
            m = dict(kcvc=kcvc, qp=np.asarray(ra[c]["qpT"]), cw1=w1, cw2=w2, pef=pef, ident=ident)
            m.update(b1c[c])
            maps.append(m)
        rb1 = _run(_prog("b1", build_phase_b1), maps)
        qr_full = cat("qrT", ra, 2)
        kskw = cat("kskwT", ra, 1)
        vsvw = cat("vsvw", ra, 0)
        vs = np.ascontiguousarray(vsvw[:, 0:64].reshape(128, 128, 64).transpose(1, 0, 2))
        vw = np.ascontiguousarray(vsvw[:, 64:128].reshape(128, 128, 64).transpose(1, 0, 2))
        negMT = cat("negMT", rb1, 1)
        oc_full = cat("ocT", rb1, 2)
        G_full = cat("G", ra, 2)
        ks = np.ascontiguousarray(kskw[0:64]); kw = np.ascontiguousarray(kskw[64:128])
        rb2 = _run(_prog("b2", build_phase_b2),
                   [dict(qr=np.ascontiguousarray(qr_full[h]), ks=ks, kw=kw, vs=vs, vw=vw, negMT=negMT,
                         oc=np.ascontiguousarray(oc_full[h]), G3=np.ascontiguousarray(G_full[3 * h:3 * h + 3]), etab=etab)
                    for h in range(NCORES)])
        sq_full = cat("sqT", ra, 2)
        sk_full = cat("skT", ra, 2)
        sv_full = cat("sv", ra, 0)
        maps = []
        for c in range(NCORES):
            hh, par = c // 2, c % 2
            sq = np.stack([sq_full[hh][:, 512 * (2 * m + par):512 * (2 * m + par) + 512] for m in range(16)], 1)
            svh = np.ascontiguousarray(sv_full[:, 128 * hh:128 * hh + 128].reshape(128, 128, 128).transpose(1, 0, 2))
            maps.append(dict(sq=np.ascontiguousarray(sq), sk=np.ascontiguousarray(sk_full[hh]), sv=svh, mk=mks[par], tri=tri, ones=ones))
        rsb = _run(_prog("sb", build_phase_sb), maps)
        onsa_full = np.stack([np.asarray(rb2[h]["onsa"]) for h in range(NCORES)], 0)
        osb_full = np.zeros((4, 128, S), bf)
        for c in range(NCORES):
            hh, par = c // 2, c % 2
            o = np.asarray(rsb[c]["osb"])
            for m in range(16):
                g = 2 * m + par
                osb_full[hh][:, 512 * g:512 * g + 512] = o[:, m, :]
        wn, ws, wo, w1f, w2f = layout_c_weights(np.asarray(w_nsa_o[l], np.float32), np.asarray(w_sb_o[l], np.float32),
                                                np.asarray(w_out[l], np.float32), np.asarray(w_ff1[l], np.float32),
                                                np.asarray(w_ff2[l], np.float32))
        g123 = np.ascontiguousarray(np.broadcast_to(ng[1:4][:, None, :], (3, 128, D)))
        maps = []
        for c in range(NCORES):
            tsl = slice(c * TOK, (c + 1) * TOK)
            maps.append(dict(x=xs[c], onsaT=np.ascontiguousarray(onsa_full[:, :, tsl]), osbT=np.ascontiguousarray(osb_full[:, :, tsl]),
                             mgT=np.asarray(ra[c]["mgT"]), g123=g123, ident=ident, wn=wn, ws=ws, wo=wo, w1=w1f, w2=w2f))
        rc_ = _run(_prog("c", build_phase_c), maps)
        xs = [np.ascontiguousarray(np.asarray(rc_[c]["xo"], np.float32)) for c in range(NCORES)]
    out = np.concatenate(xs, 0)[None].astype(np.float32)
    return out
```

```python
import numpy as np
import concourse.bass as bass
import concourse.mybir as mybir
from concourse.bass_utils import run_bass_kernel_spmd

F32 = mybir.dt.float32
BF16 = mybir.dt.bfloat16
I32 = mybir.dt.int32
AF = mybir.ActivationFunctionType
ALU = mybir.AluOpType

NCORES = 8
S = 16384
D = 1024
DEPTH = 4
TOK = S // NCORES
NSB = TOK // 512
DFF = 4096
IN_W = 4504
EPS = 1e-6
NEG = -30000.0
TWO_PI = float(2 * np.pi)

SEM_LIMIT = 30000
import os
STORE_ENG = os.environ.get("STORE_ENG", "act")
CAST_ENG = os.environ.get("CAST_ENG", "pool")
ATTACH_WAITS = os.environ.get("ATTACH_WAITS", "1") == "1"
ENGS = ("pe", "act", "dve", "pool", "sp")


class Dep:
    __slots__ = ("w", "r", "dsem", "dcnt")

    def __init__(self):
        self.w = None
        self.r = []
        self.dsem = None
        self.dcnt = 0


class Op:
    __slots__ = ("eng", "fn", "reads", "writes", "dma", "waits", "sig", "semid", "semval", "dsem", "dval", "bar")

    def __init__(self, eng, fn, reads, writes, dma):
        self.eng, self.fn, self.reads, self.writes, self.dma = eng, fn, reads, writes, dma
        self.waits = ()
        self.sig = False
        self.semid = None
        self.semval = 0
        self.dsem = None
        self.dval = 0
        self.bar = False


class _Rec:
    def __getattr__(self, name):
        return lambda *a, **k: (name, a, k)


_REC = _Rec()


class Prog:
    def __init__(self, nc):
        self.nc = nc
        self.ops = []
        self.engs = {"pe": nc.tensor, "act": nc.scalar, "dve": nc.vector, "pool": nc.gpsimd, "sp": nc.sync}
        self.n_dsem = 0
        self.dma_rr = 0
        self.stack = None

    def sb(self, name, shape, dtype):
        if self.stack is not None:
            return self.stack.enter_context(self.nc.sbuf_tensor("s_" + name, list(shape), dtype))
        return self.nc.alloc_sbuf_tensor("s_" + name, list(shape), dtype)

    def open_scope(self):
        import contextlib
        assert self.stack is None
        self.stack = contextlib.ExitStack()

    def close_scope(self):
        self.barrier()
        self.stack.close()
        self.stack = None

    def ps(self, name, shape, dtype=F32):
        return self.nc.alloc_psum_tensor("p_" + name, list(shape), dtype)

    def op(self, eng, fn, reads=(), writes=()):
        self.ops.append(Op(eng, fn(_REC), tuple(reads), tuple(writes), False))

    def dma(self, out, in_, reads=(), writes=(), prim=None, eng=None, **kw):
        if eng is None:
            eng = "sp"
        if prim is None:
            prim = (writes[0] if writes else reads[0])
        if prim.dsem is None:
            prim.dsem = self.n_dsem
            self.n_dsem += 1
        o = Op(eng, (out, in_, kw), tuple(reads), tuple(writes), True)
        prim.dcnt += 16
        o.dsem, o.dval = prim.dsem, prim.dcnt
        self.ops.append(o)

    def barrier(self):
        for e in ENGS:
            o = Op(e, None, (), (), False)
            o.bar = True
            self.ops.append(o)

    def finalize(self, final_deps=()):
        nc = self.nc
        ops = self.ops
        last_eng = {e: None for e in ENGS}
        pend_dma = []
        bar_need = None
        for i, o in enumerate(ops):
            if o.bar:
                if bar_need is None:
                    bar_need = set(j for j in last_eng.values() if j is not None) | set(pend_dma)
                o.waits = set(bar_need)
                continue
            if bar_need is not None:
                bar_need = None
                pend_dma = []
            need = set()
            for d in o.reads:
                if d.w is not None:
                    need.add(d.w)
            for d in o.writes:
                if d.w is not None:
                    need.add(d.w)
                for r in d.r:
                    need.add(r)
            need.discard(i)
            if o.dma:
                need = set(j for j in need if not (ops[j].dma and ops[j].dsem == o.dsem))
            o.waits = need
            for d in o.reads:
                d.r.append(i)
            for d in o.writes:
                d.w = i
                d.r = []
            if o.dma:
                pend_dma.append(i)
            else:
                last_eng[o.eng] = i
        fin_need = set()
        for d in final_deps:
            if d.w is not None:
                fin_need.add(d.w)
            for r in d.r:
                fin_need.add(r)
        for o in ops:
            for j in o.waits:
                if not ops[j].dma:
                    ops[j].sig = True
        for j in fin_need:
            if not ops[j].dma:
                ops[j].sig = True
        cnt = {e: 0 for e in ENGS}
        for o in ops:
            if o.sig and not o.dma:
                k = cnt[o.eng]
                cnt[o.eng] += 1
                o.semid = (o.eng, k // SEM_LIMIT)
                o.semval = (k % SEM_LIMIT) + 1
        sems = {}
        for e in ENGS:
            for ep in range((cnt[e] + SEM_LIMIT - 1) // SEM_LIMIT):
                sems[(e, ep)] = nc.alloc_semaphore(f"s_{e}_{ep}")
        dsems = [nc.alloc_semaphore(f"d_{i}") for i in range(self.n_dsem)]
        waited = {e: {} for e in ENGS}
        n_wait = 0

        def collect_waits(eng, need):
            wl = {}
            for j in need:
                p = ops[j]
                if p.dma:
                    key, val = ("d", p.dsem), p.dval
                else:
                    key, val = p.semid, p.semval
                if wl.get(key, 0) < val:
                    wl[key] = val
            res = []
            for key, val in wl.items():
                if waited[eng].get(key, 0) >= val:
                    continue
                waited[eng][key] = val
                res.append((dsems[key[1]] if key[0] == "d" else sems[key], val))
            return res

        for o in ops:
            E = self.engs[o.eng]
            wl = collect_waits(o.eng, o.waits)
            attach = None
            if wl and not o.bar and ATTACH_WAITS:
                attach = wl.pop()
            for s_, v_ in wl:
                E.wait_ge(s_, v_)
                n_wait += 1
            if o.bar:
                continue
            if o.dma:
                out, in_, kw = o.fn
                ins = E.dma_start(out=out, in_=in_, **kw)
                if attach is not None:
                    ins._wait_ge(attach[0], attach[1])
                ins.then_inc(dsems[o.dsem], 16)
            else:
                ins = getattr(E, o.fn[0])(*o.fn[1], **o.fn[2])
                if attach is not None:
                    ins._wait_ge(attach[0], attach[1])
                if o.sig:
                    ins.then_inc(sems[o.semid], 1)
        for s_, v_ in collect_waits("sp", fin_need):
            self.engs["sp"].wait_ge(s_, v_)
        self.stats = dict(n_ops=len(ops), n_wait=n_wait, n_sems=len(sems) + len(dsems))
        return self.stats


class Ring:
    def __init__(self, P, name, n, shape, dtype, psum=False):
        self.tiles = [(P.ps if psum else P.sb)(f"{name}{i}", shape, dtype) for i in range(n)]
        self.deps = [Dep() for _ in range(n)]
        self.i = 0
        self.n = n

    def next(self):
        t, d = self.tiles[self.i], self.deps[self.i]
        self.i = (self.i + 1) % self.n
        return t, d


def dram_in(nc, name, shape, dt):
    return nc.dram_tensor(name, list(shape), dt, kind="ExternalInput").ap()


def dram_out(nc, name, shape, dt):
    return nc.dram_tensor(name, list(shape), dt, kind="ExternalOutput").ap()


def _swap_cols(base):
    c = np.arange(base, base + 64)
    o = c.copy()
    o[0:8] = c[8:16]
    o[8:16] = c[0:8]
    return o


def win_col_groups():
    g = []
    for i in range(4):
        g.append(np.arange(128 * i, 128 * i + 128))
    for i in range(4):
        g.append(np.concatenate([_swap_cols(128 * i), _swap_cols(128 * i + 64)]))
    g.append(np.arange(512, 640))
    g.append(np.concatenate([np.arange(640, 704), np.arange(768, 832)]))
    g.append(np.concatenate([_swap_cols(640), _swap_cols(768)]))
    for i in range(12):
        g.append(np.concatenate([np.full(64, 896 + 2 * i), np.full(64, 896 + 2 * i + 1)]))
    for i in range(4):
        g.append(np.arange(920 + 128 * i, 920 + 128 * i + 128))
    for i in range(4):
        g.append(np.arange(1432 + 128 * i, 1432 + 128 * i + 128))
    for i in range(16):
        g.append(np.arange(2456 + 128 * i, 2456 + 128 * i + 128))
    return g


NG_FM = 47
TM_COLS = np.concatenate([np.arange(704, 768), np.arange(832, 896), np.arange(1944, 2456)])


def layout_w_in(w):
    groups = win_col_groups()
    fm = np.stack([w[:, c].reshape(8, 128, 128).transpose(1, 0, 2) for c in groups])
    tm = w[:, TM_COLS].reshape(8, 128, 640).transpose(1, 0, 2)
    return np.ascontiguousarray(fm), np.ascontiguousarray(tm)


def rope_consts():
    half = 8
    inv = np.power(np.float32(500000.0), np.arange(half, dtype=np.float32) * np.float32(-2.0 / 16)).astype(np.float32)
    c = np.zeros((128, 2), np.float32)
    for p in range(128):
        d = p % 64
        if d < 8:
            c[p, 0] = inv[d]
            c[p, 1] = -1.0
        elif d < 16:
            c[p, 0] = inv[d - 8]
            c[p, 1] = 1.0
    return c


def emit_rms_rstd(P, ss, dss, rstd, drstd, n):
    P.op("dve", lambda E: E.tensor_scalar(out=rstd[:, 0:n], in0=ss[:, 0:n], scalar1=1.0 / D, scalar2=EPS,
                                          op0=ALU.mult, op1=ALU.add), reads=[dss], writes=[drstd])
    P.op("act", lambda E: E.activation(out=rstd[:, 0:n], in_=rstd[:, 0:n], func=AF.Sqrt), reads=[drstd], writes=[drstd])
    P.op("dve", lambda E: E.reciprocal(out=rstd[:, 0:n], in_=rstd[:, 0:n]), reads=[drstd], writes=[drstd])


def emit_sin_table(P, out, dout, ang, dang, tmp_i, tmp_f, dtmp, shift):
    P.op("dve", lambda E: E.tensor_scalar(out=out, in0=ang[:], scalar1=float(shift), scalar2=None, op0=ALU.add),
         reads=[dang], writes=[dout])
    P.op("dve", lambda E: E.tensor_scalar(out=tmp_i[:], in0=out, scalar1=1.0 / TWO_PI, scalar2=None, op0=ALU.mult),
         reads=[dout], writes=[dtmp])
    P.op("dve", lambda E: E.tensor_copy(out=tmp_f[:], in_=tmp_i[:]), reads=[dtmp], writes=[dtmp])
    P.op("dve", lambda E: E.scalar_tensor_tensor(out=out, in0=tmp_f[:], scalar=-TWO_PI, in1=out, op0=ALU.mult,
                                                 op1=ALU.add), reads=[dtmp, dout], writes=[dout])
    P.op("dve", lambda E: E.tensor_scalar(out=tmp_f[:], in0=out, scalar1=float(np.pi), scalar2=-TWO_PI,
                                          op0=ALU.is_gt, op1=ALU.mult), reads=[dout], writes=[dtmp])
    P.op("dve", lambda E: E.tensor_tensor(out=out, in0=out, in1=tmp_f[:], op=ALU.add), reads=[dout, dtmp],
         writes=[dout])
    P.op("dve", lambda E: E.tensor_scalar(out=tmp_f[:], in0=out, scalar1=float(-np.pi), scalar2=TWO_PI,
                                          op0=ALU.is_lt, op1=ALU.mult), reads=[dout], writes=[dtmp])
    P.op("dve", lambda E: E.tensor_tensor(out=out, in0=out, in1=tmp_f[:], op=ALU.add), reads=[dout, dtmp],
         writes=[dout])
    P.op("act", lambda E: E.activation(out=out, in_=out, func=AF.Sin), reads=[dout], writes=[dout])


def emit_norm_transpose(P, xt, dx, a, rstd, drstd, gbc, dg, hb_ring, ident, dident, ptr, dptr, hT, dhT):
    hb, dhb = hb_ring.next()
    P.op("dve", lambda E: E.scalar_tensor_tensor(out=hb[:], in0=xt[:, a, :], scalar=rstd[:, a:a + 1], in1=gbc[:],
                                                 op0=ALU.mult, op1=ALU.mult), reads=[dx, drstd, dg], writes=[dhb])
    for kc in range(8):
        P.op("pe", lambda E, kc=kc: E.transpose(ptr[:, kc * 128:(kc + 1) * 128], hb[:, kc * 128:(kc + 1) * 128], ident[:]),
             reads=[dhb, dident], writes=[dptr])
    P.op("act", lambda E: E.activation(out=hT[:, :, a * 128:(a + 1) * 128],
                                       in_=ptr[:].rearrange("p (k t) -> p k t", k=8), func=AF.Copy),
         reads=[dptr], writes=[dhT])


def build_phase_a(dbg=0):
    nc = bass.Bass("TRN2", target_bir_lowering=False)
    x = dram_in(nc, "x", [TOK, D], F32)
    pos = dram_in(nc, "pos", [128, TOK], I32)
    gbc_in = dram_in(nc, "gbc", [128, D], F32)
    rc_in = dram_in(nc, "rc", [128, 2], F32)
    ident_in = dram_in(nc, "ident", [128, 128], F32)
    wfm = dram_in(nc, "wfm", [NG_FM, 128, 8, 128], F32)
    wtm = dram_in(nc, "wtm", [128, 8, 640], F32)
    o_qp = dram_out(nc, "qpT", [8, 64, TOK], BF16)
    o_qr = dram_out(nc, "qrT", [8, 64, TOK], BF16)
    o_kcvc = dram_out(nc, "kcvcT", [128, TOK], BF16)
    o_kskw = dram_out(nc, "kskwT", [128, TOK], BF16)
    o_g = dram_out(nc, "G", [24, 64, TOK], BF16)
    o_sq = dram_out(nc, "sqT", [4, 128, TOK], BF16)
    o_sk = dram_out(nc, "skT", [4, 128, TOK], BF16)
    o_mg = dram_out(nc, "mgT", [16, 128, TOK], BF16)
    o_vv = dram_out(nc, "vsvw", [TOK, 128], BF16)
    o_sv = dram_out(nc, "sv", [TOK, 512], BF16)
    P = Prog(nc)
    fin = []
    NT = TOK // 128

    xt = P.sb("xt", [128, NT, D], F32); dx = Dep()
    for a in range(NT):
        P.dma(xt[:, a, :], x[a * 128:(a + 1) * 128, :], writes=[dx])
    gbc = P.sb("gbc", [128, D], F32); dg = Dep()
    P.dma(gbc[:], gbc_in, writes=[dg])
    rc = P.sb("rc", [128, 2], F32); drc = Dep()
    P.dma(rc[:], rc_in, writes=[drc])
    idf = P.sb("idf", [128, 128], F32); didf = Dep()
    P.dma(idf[:], ident_in, writes=[didf])
    ident = P.sb("ident", [128, 128], BF16); dident = Dep()
    P.op("dve", lambda E: E.tensor_copy(out=ident[:], in_=idf[:]), reads=[didf], writes=[dident])
    posi = P.sb("posi", [128, TOK], I32); dposi = Dep()
    P.dma(posi[:], pos, writes=[dposi])

    ang = P.sb("ang", [128, 512], F32); dang = Dep()
    tmp_i = P.sb("tmp_i", [128, 512], I32); tmp_f = P.sb("tmp_f", [128, 512], F32); dtmp = Dep()
    Ct = P.sb("Ct", [128, TOK], F32); dC = Dep()
    St = P.sb("St", [128, TOK], F32); dS = Dep()
    for sbi in range(NSB):
        sl = slice(sbi * 512, (sbi + 1) * 512)
        P.op("dve", lambda E, sl=sl: E.tensor_copy(out=ang[:], in_=posi[:, sl]), reads=[dposi], writes=[dang])
        P.op("dve", lambda E: E.tensor_scalar(out=ang[:], in0=ang[:], scalar1=rc[:, 0:1], scalar2=None, op0=ALU.mult),
             reads=[dang, drc], writes=[dang])
        emit_sin_table(P, Ct[:, sl], dC, ang, dang, tmp_i, tmp_f, dtmp, np.pi / 2)
        emit_sin_table(P, St[:, sl], dS, ang, dang, tmp_i, tmp_f, dtmp, 0.0)
    P.op("dve", lambda E: E.tensor_scalar(out=St[:], in0=St[:], scalar1=rc[:, 1:2], scalar2=None, op0=ALU.mult),
         reads=[dS, drc], writes=[dS])

    if dbg == 1:
        o_dbg = dram_out(nc, "dbg", [128, TOK], F32)
        dd = Dep()
        P.dma(o_dbg, Ct[:], reads=[dC], writes=[dd])
        o_dbg2 = dram_out(nc, "dbg2", [128, TOK], F32)
        P.dma(o_dbg2, St[:], reads=[dS], writes=[dd])
        return nc, P.finalize(final_deps=[dd])
    ss = P.sb("ss", [128, NT], F32); dss = Dep()
    rstd = P.sb("rstd", [128, NT], F32); drstd = Dep()
    junk = P.sb("junk", [128, D], BF16); djunk = Dep()
    for a in range(NT):
        P.op("act", lambda E, a=a: E.activation(out=junk[:], in_=xt[:, a, :], func=AF.Square, accum_out=ss[:, a:a + 1]),
             reads=[dx], writes=[djunk, dss])
    emit_rms_rstd(P, ss, dss, rstd, drstd, NT)

    hT = P.sb("hT", [128, 8, TOK], BF16); dhT = Dep()
    hb_ring = Ring(P, "hb", 2, [128, D], BF16)
    ptr = P.ps("ptr", [128, 1024], BF16); dptr = Dep()
    for a in range(NT):
        emit_norm_transpose(P, xt, dx, a, rstd, drstd, gbc, dg, hb_ring, ident, dident, ptr, dptr, hT, dhT)

    if dbg == 2:
        o_dbg = dram_out(nc, "dbg", [128, 8, TOK], BF16)
        dd = Dep()
        P.dma(o_dbg, hT[:], reads=[dhT], writes=[dd])
        return nc, P.finalize(final_deps=[dd])
    wst = Ring(P, "wst", 2, [128, 8, 128], F32)
    wbf = Ring(P, "wbf", 3, [128, 8, 128], BF16)
    pacc = Ring(P, "pacc", 4, [128, 512], F32, psum=True)
    obuf = Ring(P, "obuf", 4, [128, 512], BF16)
    t1r = Ring(P, "t1r", 2, [128, 512], F32)
    t2r = Ring(P, "t2r", 2, [128, 512], F32)

    def load_group(gi):
        st, dst = wst.next()
        P.dma(st[:], wfm[gi], writes=[dst])
        wb, dwb = wbf.next()
        P.op(CAST_ENG, lambda E: E.tensor_copy(out=wb[:], in_=st[:]), reads=[dst], writes=[dwb])
        return wb, dwb

    def mm_group(wb, dwb, sbi):
        pt, dpt = pacc.next()
        for kc in range(8):
            P.op("pe", lambda E, kc=kc: E.matmul(pt[:], lhsT=wb[:, kc, :], rhs=hT[:, kc, sbi * 512:(sbi + 1) * 512],
                                                 start=(kc == 0), stop=(kc == 7)), reads=[dwb, dhT], writes=[dpt])
        return pt, dpt

    def store(ob, dob, dst_ap):
        d = Dep()
        P.dma(dst_ap, ob, reads=[dob], writes=[d], prim=dob, eng=STORE_ENG)
        fin.append(dob)

    def plain_group(gi, func, scale, dsts):
        wb, dwb = load_group(gi)
        for sbi in range(NSB):
            pt, dpt = mm_group(wb, dwb, sbi)
            ob, dob = obuf.next()
            P.op("act", lambda E: E.activation(out=ob[:], in_=pt[:], func=func, scale=scale), reads=[dpt], writes=[dob])
            for lo, hi, ap in dsts(sbi):
                store(ob[lo:hi, :], dob, ap)

    def rope_group(gp, gs, scale, dsts_r, dsts_p):
        wp, dwp = load_group(gp)
        ws, dws = load_group(gs)
        for sbi in range(NSB):
            pp, dpp = mm_group(wp, dwp, sbi)
            pq, dpq = mm_group(ws, dws, sbi)
            sl = slice(sbi * 512, (sbi + 1) * 512)
            t1, dt1 = t1r.next()
            t2, dt2 = t2r.next()
            P.op("dve", lambda E: E.tensor_tensor(out=t1[:], in0=pp[:], in1=Ct[:, sl], op=ALU.mult), reads=[dpp, dC], writes=[dt1])
            P.op("dve", lambda E: E.tensor_tensor(out=t2[:], in0=pq[:], in1=St[:, sl], op=ALU.mult), reads=[dpq, dS], writes=[dt2])
            P.op("dve", lambda E: E.tensor_tensor(out=t1[:], in0=t1[:], in1=t2[:], op=ALU.add), reads=[dt1, dt2], writes=[dt1])
            ob, dob = obuf.next()
            P.op("act", lambda E: E.activation(out=ob[:], in_=t1[:], func=AF.Copy, scale=scale), reads=[dt1], writes=[dob])
            for lo, hi, ap in dsts_r(sbi):
                store(ob[lo:hi, :], dob, ap)
            if dsts_p is not None:
                ob2, dob2 = obuf.next()
                P.op("act", lambda E: E.activation(out=ob2[:], in_=pp[:], func=AF.Copy, scale=scale), reads=[dpp], writes=[dob2])
                for lo, hi, ap in dsts_p(sbi):
                    store(ob2[lo:hi, :], dob2, ap)

    def tsl(sbi):
        return slice(sbi * 512, (sbi + 1) * 512)

    for i in range(0 if dbg == 5 else 4):
        rope_group(i, 4 + i, 0.125,
                   lambda sbi, i=i: [(0, 64, o_qr[2 * i, :, tsl(sbi)]), (64, 128, o_qr[2 * i + 1, :, tsl(sbi)])],
                   lambda sbi, i=i: [(0, 64, o_qp[2 * i, :, tsl(sbi)]), (64, 128, o_qp[2 * i + 1, :, tsl(sbi)])])
    if dbg == 3:
        return nc, P.finalize(final_deps=fin)
    plain_group(8, AF.Copy, 1.0, lambda sbi: [(0, 128, o_kcvc[:, tsl(sbi)])])
    if dbg in (4, 5):
        return nc, P.finalize(final_deps=fin)
    rope_group(9, 10, 1.0, lambda sbi: [(0, 128, o_kskw[:, tsl(sbi)])], None)
    for i in range(12):
        plain_group(11 + i, AF.Sigmoid, 1.0,
                    lambda sbi, i=i: [(0, 64, o_g[2 * i, :, tsl(sbi)]), (64, 128, o_g[2 * i + 1, :, tsl(sbi)])])
    for i in range(4):
        plain_group(23 + i, AF.Copy, float(128 ** -0.5), lambda sbi, i=i: [(0, 128, o_sq[i, :, tsl(sbi)])])
    for i in range(4):
        plain_group(27 + i, AF.Copy, 1.0, lambda sbi, i=i: [(0, 128, o_sk[i, :, tsl(sbi)])])
    for i in range(16):
        plain_group(31 + i, AF.Sigmoid, 1.0, lambda sbi, i=i: [(0, 128, o_mg[i, :, tsl(sbi)])])

    wtst = Ring(P, "wtst", 2, [128, 640], F32)
    wtb = P.sb("wtb", [128, 8, 640], BF16); dwtb = Dep()
    for kc in range(8):
        st_, dst_ = wtst.next()
        P.dma(st_[:], wtm[:, kc, :], writes=[dst_])
        P.op("pool", lambda E, kc=kc, st_=st_: E.tensor_copy(out=wtb[:, kc, :], in_=st_[:]), reads=[dst_], writes=[dwtb])
    otm = Ring(P, "otm", 2, [128, 640], BF16)
    for a in range(NT):
        p1, dp1 = pacc.next()
        p2, dp2 = pacc.next()
        for kc in range(8):
            P.op("pe", lambda E, kc=kc: E.matmul(p1[:, 0:128], lhsT=hT[:, kc, a * 128:(a + 1) * 128], rhs=wtb[:, kc, 0:128],
                                                 start=(kc == 0), stop=(kc == 7)), reads=[dwtb, dhT], writes=[dp1])
        for kc in range(8):
            P.op("pe", lambda E, kc=kc: E.matmul(p2[:], lhsT=hT[:, kc, a * 128:(a + 1) * 128], rhs=wtb[:, kc, 128:640],
                                                 start=(kc == 0), stop=(kc == 7)), reads=[dwtb, dhT], writes=[dp2])
        ot, dot = otm.next()
        P.op("act", lambda E: E.activation(out=ot[:, 0:128], in_=p1[:, 0:128], func=AF.Copy), reads=[dp1], writes=[dot])
        P.op("dve", lambda E: E.tensor_copy(out=ot[:, 128:640], in_=p2[:]), reads=[dp2], writes=[dot])
        store(ot[:, 0:128], dot, o_vv[a * 128:(a + 1) * 128, :])
        store(ot[:, 128:640], dot, o_sv[a * 128:(a + 1) * 128, :])

    st = P.finalize(final_deps=fin)
    return nc, st


def layout_c_weights(w_nsa_o, w_sb_o, w_out, w_ff1, w_ff2):
    wn = np.ascontiguousarray(w_nsa_o.reshape(8, 64, D).transpose(1, 0, 2))
    ws = np.ascontiguousarray(w_sb_o.reshape(4, 128, D).transpose(1, 0, 2))
    wo = np.ascontiguousarray(w_out.reshape(8, 128, D).transpose(1, 0, 2))
    w1 = np.ascontiguousarray(w_ff1.reshape(8, 128, DFF).transpose(1, 0, 2))
    w2 = np.ascontiguousarray(w_ff2.reshape(32, 128, D).transpose(1, 0, 2))
    return wn, ws, wo, w1, w2


def build_phase_c():
    nc = bass.Bass("TRN2", target_bir_lowering=False)
    x = dram_in(nc, "x", [TOK, D], F32)
    onsa = dram_in(nc, "onsaT", [8, 64, TOK], BF16)
    osb = dram_in(nc, "osbT", [4, 128, TOK], BF16)
    mg = dram_in(nc, "mgT", [16, 128, TOK], BF16)
    g_in = dram_in(nc, "g123", [3, 128, D], F32)
    ident_in = dram_in(nc, "ident", [128, 128], F32)
    wn_in = dram_in(nc, "wn", [64, 8, D], F32)
    ws_in = dram_in(nc, "ws", [128, 4, D], F32)
    wo_in = dram_in(nc, "wo", [128, 8, D], F32)
    w1_in = dram_in(nc, "w1", [128, 8, DFF], F32)
    w2_in = dram_in(nc, "w2", [128, 32, D], F32)
    xo = dram_out(nc, "xo", [TOK, D], F32)
    P = Prog(nc)
    fin = []
    NT = TOK // 128

    xt = P.sb("xt", [128, NT, D], F32); dx = Dep()
    for a in range(NT):
        P.dma(xt[:, a, :], x[a * 128:(a + 1) * 128, :], writes=[dx])
    g123 = P.sb("g123", [128, 3, D], F32); dg = Dep()
    for i in range(3):
        P.dma(g123[:, i, :], g_in[i], writes=[dg])
    idf = P.sb("idf", [128, 128], F32); didf = Dep()
    P.dma(idf[:], ident_in, writes=[didf])
    ident = P.sb("ident", [128, 128], BF16); dident = Dep()
    P.op("dve", lambda E: E.tensor_copy(out=ident[:], in_=idf[:]), reads=[didf], writes=[dident])
    ss = P.sb("ss", [128, 4], F32); dss = Dep()
    rstd = P.sb("rstd", [128, 4], F32); drstd = Dep()
    junk = P.sb("junk", [128, 512], BF16); djunk = Dep()
    pacc = Ring(P, "pacc", 7, [128, 512], F32, psum=True)
    ptr = P.ps("ptr", [128, 1024], BF16); dptr = Dep()
    stg = Ring(P, "stg", 2, [128, 2048], F32)

    def rms_from_psum(p0, d0, p1, d1):
        P.op("act", lambda E: E.activation(out=junk[:], in_=p0[:], func=AF.Square, accum_out=ss[:, 0:1]), reads=[d0], writes=[djunk, dss])
        P.op("act", lambda E: E.activation(out=junk[:], in_=p1[:], func=AF.Square, accum_out=ss[:, 1:2]), reads=[d1], writes=[djunk, dss])
        P.op("dve", lambda E: E.tensor_tensor(out=ss[:, 2:3], in0=ss[:, 0:1], in1=ss[:, 1:2], op=ALU.add), reads=[dss], writes=[dss])
        P.op("dve", lambda E: E.tensor_scalar(out=rstd[:, 0:1], in0=ss[:, 2:3], scalar1=1.0 / D, scalar2=EPS, op0=ALU.mult, op1=ALU.add), reads=[dss], writes=[drstd])
        P.op("act", lambda E: E.activation(out=rstd[:, 0:1], in_=rstd[:, 0:1], func=AF.Sqrt), reads=[drstd], writes=[drstd])
        P.op("dve", lambda E: E.reciprocal(out=rstd[:, 0:1], in_=rstd[:, 0:1]), reads=[drstd], writes=[drstd])

    P.open_scope()
    wn = P.sb("wn", [64, 8, D], BF16); dwn = Dep()
    ws = P.sb("ws", [128, 4, D], BF16); dws = Dep()
    wo = P.sb("wo", [128, 8, D], BF16); dwo = Dep()
    for h in range(8):
        st, dst = stg.next()
        P.dma(st[0:64, 0:D], wn_in[:, h, :], writes=[dst])
        P.op("pool", lambda E: E.tensor_copy(out=wn[:, h, :], in_=st[0:64, 0:D]), reads=[dst], writes=[dwn])
    for h in range(4):
        st, dst = stg.next()
        P.dma(st[:, 0:D], ws_in[:, h, :], writes=[dst])
        P.op("pool", lambda E: E.tensor_copy(out=ws[:, h, :], in_=st[:, 0:D]), reads=[dst], writes=[dws])
    for h in range(8):
        st, dst = stg.next()
        P.dma(st[:, 0:D], wo_in[:, h, :], writes=[dst])
        P.op("pool", lambda E: E.tensor_copy(out=wo[:, h, :], in_=st[:, 0:D]), reads=[dst], writes=[dwo])
    on = P.sb("on", [64, 8, 512], BF16); don = Dep()
    os_ = P.sb("os", [128, 4, 512], BF16); dos = Dep()
    mgt = P.sb("mgt", [128, 16, 512], BF16); dmg = Dep()
    merged = P.sb("merged", [128, 8, 512], BF16); dmer = Dep()
    t1r = Ring(P, "t1r", 2, [128, 512], F32)
    t2r = Ring(P, "t2r", 2, [128, 512], F32)
    for j in range(NSB):
        sl = slice(j * 512, (j + 1) * 512)
        for h in range(8):
            P.dma(on[:, h, :], onsa[h, :, sl], writes=[don])
        for h in range(4):
            P.dma(os_[:, h, :], osb[h, :, sl], writes=[dos])
        for h in range(16):
            P.dma(mgt[:, h, :], mg[h, :, sl], writes=[dmg])
        for dc in range(8):
            py, dpy = pacc.next()
            pz, dpz = pacc.next()
            for h in range(8):
                P.op("pe", lambda E: E.matmul(py[:], lhsT=wn[:, h, dc * 128:(dc + 1) * 128], rhs=on[:, h, :], start=(h == 0), stop=(h == 7)),
                     reads=[dwn, don], writes=[dpy])
            for h in range(4):
                P.op("pe", lambda E: E.matmul(pz[:], lhsT=ws[:, h, dc * 128:(dc + 1) * 128], rhs=os_[:, h, :], start=(h == 0), stop=(h == 3)),
                     reads=[dws, dos], writes=[dpz])
            t1, dt1 = t1r.next()
            t2, dt2 = t2r.next()
            P.op("dve", lambda E: E.tensor_tensor(out=t1[:], in0=py[:], in1=mgt[:, dc, :], op=ALU.mult), reads=[dpy, dmg], writes=[dt1])
            P.op("dve", lambda E: E.tensor_tensor(out=t2[:], in0=pz[:], in1=mgt[:, 8 + dc, :], op=ALU.mult), reads=[dpz, dmg], writes=[dt2])
            P.op("dve", lambda E: E.tensor_tensor(out=merged[:, dc, :], in0=t1[:], in1=t2[:], op=ALU.add), reads=[dt1, dt2], writes=[dmer])
        for tt in range(4):
            a = 4 * j + tt
            pm = []
            for half in range(2):
                p_, dp_ = pacc.next()
                for dc in range(8):
                    P.op("pe", lambda E: E.matmul(p_[:], lhsT=merged[:, dc, tt * 128:(tt + 1) * 128], rhs=wo[:, dc, half * 512:(half + 1) * 512],
                                                 start=(dc == 0), stop=(dc == 7)), reads=[dmer, dwo], writes=[dp_])
                pm.append((p_, dp_))
            rms_from_psum(pm[0][0], pm[0][1], pm[1][0], pm[1][1])
            for half in range(2):
                t1, dt1 = t1r.next()
                hs = slice(half * 512, (half + 1) * 512)
                P.op("dve", lambda E: E.scalar_tensor_tensor(out=t1[:], in0=pm[half][0][:], scalar=rstd[:, 0:1], in1=g123[:, 0, hs],
                                                             op0=ALU.mult, op1=ALU.mult), reads=[pm[half][1], drstd, dg], writes=[dt1])
                P.op("dve", lambda E: E.tensor_tensor(out=xt[:, a, hs], in0=xt[:, a, hs], in1=t1[:], op=ALU.add), reads=[dx, dt1], writes=[dx])
    P.close_scope()

    P.open_scope()
    hfT = P.sb("hfT", [128, 8, 512], BF16); dhT = Dep()
    h1 = P.sb("h1", [128, 32, 512], BF16); dh1 = Dep()
    hb_ring = Ring(P, "hb", 2, [128, D], BF16)
    w1b = Ring(P, "w1b", 2, [128, 8, 256], BF16)
    w2b = Ring(P, "w2b", 2, [128, D], BF16)
    rl = Ring(P, "rl", 2, [128, 512], F32)
    ss4 = P.sb("ss4", [128, 4], F32); dss4 = Dep()
    rs4 = P.sb("rs4", [128, 4], F32); drs4 = Dep()
    junk2 = P.sb("junk2", [128, D], BF16); djunk2 = Dep()
    t1r = Ring(P, "t1f", 2, [128, 512], F32)
    for j in range(NSB):
        for tt in range(4):
            a = 4 * j + tt
            P.op("act", lambda E: E.activation(out=junk2[:], in_=xt[:, a, :], func=AF.Square, accum_out=ss4[:, tt:tt + 1]),
                 reads=[dx], writes=[djunk2, dss4])
        emit_rms_rstd(P, ss4, dss4, rs4, drs4, 4)
        for tt in range(4):
            a = 4 * j + tt
            hb, dhb = hb_ring.next()
            P.op("dve", lambda E: E.scalar_tensor_tensor(out=hb[:], in0=xt[:, a, :], scalar=rs4[:, tt:tt + 1], in1=g123[:, 1, :],
                                                         op0=ALU.mult, op1=ALU.mult), reads=[dx, drs4, dg], writes=[dhb])
            for kc in range(8):
                P.op("pe", lambda E: E.transpose(ptr[:, kc * 128:(kc + 1) * 128], hb[:, kc * 128:(kc + 1) * 128], ident[:]),
                     reads=[dhb, dident], writes=[dptr])
            P.op("act", lambda E: E.activation(out=hfT[:, :, tt * 128:(tt + 1) * 128], in_=ptr[:].rearrange("p (k t) -> p k t", k=8),
                                               func=AF.Copy), reads=[dptr], writes=[dhT])
        for fg in range(16):
            st, dst = stg.next()
            P.dma(st[:].rearrange("p (k c) -> p k c", k=8), w1_in[:, :, fg * 256:(fg + 1) * 256], writes=[dst])
            wb, dwb = w1b.next()
            P.op("pool", lambda E: E.tensor_copy(out=wb[:], in_=st[:].rearrange("p (k c) -> p k c", k=8)), reads=[dst], writes=[dwb])
            for f2 in range(2):
                fc = fg * 2 + f2
                ph, dph = pacc.next()
                for kc in range(8):
                    P.op("pe", lambda E: E.matmul(ph[:], lhsT=wb[:, kc, f2 * 128:(f2 + 1) * 128], rhs=hfT[:, kc, :], start=(kc == 0), stop=(kc == 7)),
                         reads=[dwb, dhT], writes=[dph])
                r_, dr_ = rl.next()
                P.op("act", lambda E: E.activation(out=r_[:], in_=ph[:], func=AF.Relu), reads=[dph], writes=[dr_])
                P.op("dve", lambda E: E.tensor_tensor(out=h1[:, fc, :], in0=r_[:], in1=r_[:], op=ALU.mult), reads=[dr_], writes=[dh1])
        for tp in range(2):
            accs = [pacc.next() for _ in range(4)]
            for fc in range(32):
                st, dst = stg.next()
                P.dma(st[:, 0:D], w2_in[:, fc, :], writes=[dst])
                wb, dwb = w2b.next()
                P.op("pool", lambda E: E.tensor_copy(out=wb[:], in_=st[:, 0:D]), reads=[dst], writes=[dwb])
                for t2 in range(2):
                    tt = tp * 2 + t2
                    for half in range(2):
                        p_, dp_ = accs[t2 * 2 + half]
                        P.op("pe", lambda E: E.matmul(p_[:], lhsT=h1[:, fc, tt * 128:(tt + 1) * 128], rhs=wb[:, half * 512:(half + 1) * 512],
                                                     start=(fc == 0), stop=(fc == 31)), reads=[dh1, dwb], writes=[dp_])
            for t2 in range(2):
                tt = tp * 2 + t2
                a = 4 * j + tt
                (p0, d0), (p1, d1) = accs[t2 * 2], accs[t2 * 2 + 1]
                rms_from_psum(p0, d0, p1, d1)
                for half, (p_, dp_) in enumerate(((p0, d0), (p1, d1))):
                    t1, dt1 = t1r.next()
                    hs = slice(half * 512, (half + 1) * 512)
                    P.op("dve", lambda E: E.scalar_tensor_tensor(out=t1[:], in0=p_[:], scalar=rstd[:, 0:1], in1=g123[:, 2, hs],
                                                                 op0=ALU.mult, op1=ALU.mult), reads=[dp_, drstd, dg], writes=[dt1])
                    P.op("dve", lambda E: E.tensor_tensor(out=xt[:, a, hs], in0=xt[:, a, hs], in1=t1[:], op=ALU.add), reads=[dx, dt1], writes=[dx])
                dd = Dep()
                P.dma(xo[a * 128:(a + 1) * 128, :], xt[:, a, :], reads=[dx], writes=[dd])
                fin.append(dd)
    P.close_scope()
    st = P.finalize(final_deps=fin)
    return nc, st


def sb_mask_table(parity):
    p = np.arange(128)[:, None, None]
    i = np.arange(8)[None, :, None]
    f = np.arange(512)[None, None, :]
    return (512 * parity + f - 128 * i - p > 0)


def build_phase_sb(n_m=16):
    nc = bass.Bass("TRN2", target_bir_lowering=False)
    sq_in = dram_in(nc, "sq", [128, 16, 512], BF16)
    sk_in = dram_in(nc, "sk", [128, S], BF16)
    sv_in = dram_in(nc, "sv", [128, 128, 128], BF16)
    mk_in = dram_in(nc, "mk", [128, 8, 512], BF16)
    tri_in = dram_in(nc, "tri", [128, 128], BF16)
    ones_in = dram_in(nc, "ones", [128, 128], BF16)
    out = dram_out(nc, "osb", [128, 16, 512], BF16)
    P = Prog(nc)
    fin = []
    sq = P.sb("sq", [128, 16, 512], BF16); dsq = Dep()
    P.dma(sq[:], sq_in, writes=[dsq])
    sk = P.sb("sk", [128, S], BF16); dsk = Dep()
    nsk = P.sb("nsk", [128, S], BF16); dnsk = Dep()
    for i in range(4):
        sl = slice(i * 4096, (i + 1) * 4096)
        P.dma(sk[:, sl], sk_in[:, sl], writes=[dsk])
    for i in range(4):
        sl = slice(i * 4096, (i + 1) * 4096)
        P.op("pool", lambda E: E.tensor_scalar(out=nsk[:, sl], in0=sk[:, sl], scalar1=-1.0, scalar2=None, op0=ALU.mult), reads=[dsk], writes=[dnsk])
    sv = P.sb("sv", [128, 128, 128], BF16); dsv = Dep()
    for i in range(4):
        P.dma(sv[:, i * 32:(i + 1) * 32, :], sv_in[:, i * 32:(i + 1) * 32, :], writes=[dsv])
    mk = P.sb("mk", [128, 8, 512], BF16); dmk = Dep()
    P.dma(mk[:], mk_in, writes=[dmk])
    tri = P.sb("tri", [128, 128], BF16); dtri = Dep()
    P.dma(tri[:], tri_in, writes=[dtri])
    ones = P.sb("ones", [128, 128], BF16); dones = Dep()
    P.dma(ones[:], ones_in, writes=[dones])
    pzr = Ring(P, "pz", 2, [128, 512], F32, psum=True)
    pcr = Ring(P, "pc", 2, [128, 512], F32, psum=True)
    por = Ring(P, "po", 2, [128, 512], F32, psum=True)
    er = Ring(P, "e", 2, [128, 512], F32)
    spr = Ring(P, "sp", 4, [128, 512], BF16)
    ar = Ring(P, "a", 3, [128, 512], BF16)
    R = P.sb("R", [128, 512], F32); dR = Dep()
    rbr = Ring(P, "rb", 4, [128, 512], BF16)
    obr = Ring(P, "ob", 2, [128, 512], BF16)
    units = []
    for m in range(n_m):
        L = 8 * (m + 1)
        for idx, kc in enumerate(reversed(range(L))):
            units.append((m, L, idx, kc))
    state = {}
    pos_ = {}

    def stage1(u):
        m, L, idx, kc = u
        ti = kc - (L - 8)
        first, last = idx == 0, idx == L - 1
        ks = slice(kc * 128, (kc + 1) * 128)
        pz, dpz = pzr.next()
        P.op("pe", lambda E: E.matmul(pz[:], lhsT=sk[:, ks], rhs=sq[:, m, :], start=True, stop=True), reads=[dsk, dsq], writes=[dpz])
        e, de = er.next()
        P.op("act", lambda E: E.activation(out=e[:], in_=pz[:], func=AF.Exp), reads=[dpz], writes=[de])
        sp, dsp = spr.next()
        P.op("act", lambda E: E.activation(out=sp[:], in_=e[:], func=AF.Ln, bias=1.0), reads=[de], writes=[dsp])
        if ti >= 0:
            P.op("dve", lambda E: E.tensor_tensor(out=sp[:], in0=sp[:], in1=mk[:, ti, :], op=ALU.mult), reads=[dsp, dmk], writes=[dsp])
        prev_rb = state.get("rb")
        if not last:
            if first:
                P.op("dve", lambda E: E.tensor_copy(out=R[:], in_=sp[:]), reads=[dsp], writes=[dR])
            else:
                P.op("dve", lambda E: E.tensor_tensor(out=R[:], in0=R[:], in1=sp[:], op=ALU.add), reads=[dR, dsp], writes=[dR])
            rb, drb = rbr.next()
            P.op("pool", lambda E: E.tensor_copy(out=rb[:], in_=R[:]), reads=[dR], writes=[drb])
            state["rb"] = (rb, drb)
        return (sp, dsp, None if first else prev_rb)

    def stage2(u, s1):
        m, L, idx, kc = u
        sp, dsp, prb = s1
        ti = kc - (L - 8)
        first, last = idx == 0, idx == L - 1
        ks = slice(kc * 128, (kc + 1) * 128)
        if first:
            pos_[m] = por.next()
        po, dpo = pos_[m]
        pc, dpc = pcr.next()
        P.op("pe", lambda E: E.matmul(pc[:], lhsT=tri[:], rhs=sp[:], start=True, stop=False), reads=[dtri, dsp], writes=[dpc])
        if prb is not None:
            rb, drb = prb
            P.op("pe", lambda E: E.matmul(pc[:], lhsT=ones[:], rhs=rb[:], start=False, stop=False), reads=[dones, drb], writes=[dpc])
        P.op("pe", lambda E: E.matmul(pc[:], lhsT=nsk[:, ks], rhs=sq[:, m, :], start=False, stop=True), reads=[dnsk, dsq], writes=[dpc])
        a, da = ar.next()
        P.op("act", lambda E: E.activation(out=a[:], in_=pc[:], func=AF.Exp, scale=-1.0), reads=[dpc], writes=[da])
        if ti >= 0:
            P.op("dve", lambda E: E.tensor_tensor(out=a[:], in0=a[:], in1=mk[:, ti, :], op=ALU.mult), reads=[da, dmk], writes=[da])
        P.op("pe", lambda E: E.matmul(po[:], lhsT=sv[:, kc, :], rhs=a[:], start=first, stop=last), reads=[dsv, da], writes=[dpo])
        if last:
            ob, dob = obr.next()
            P.op("act", lambda E: E.activation(out=ob[:], in_=po[:], func=AF.Copy), reads=[dpo], writes=[dob])
            dd = Dep()
            P.dma(out[:, m, :], ob[:], reads=[dob], writes=[dd], prim=dob)
            fin.append(dob)

    cur = stage1(units[0])
    for k, u in enumerate(units):
        nxt = stage1(units[k + 1]) if k + 1 < len(units) else None
        stage2(u, cur)
        cur = nxt
    st = P.finalize(final_deps=fin)
    return nc, st


def etab_const():
    key = np.arange(S)
    r = np.arange(64)[:, None]
    return ((key[None, :] // 64) % 64 == r)


def build_phase_b2(n_g=32):
    nc = bass.Bass("TRN2", target_bir_lowering=False)
    qr_in = dram_in(nc, "qr", [64, S], BF16)
    ks_in = dram_in(nc, "ks", [64, S], BF16)
    kw_in = dram_in(nc, "kw", [64, S], BF16)
    vs_in = dram_in(nc, "vs", [128, 128, 64], BF16)
    vw_in = dram_in(nc, "vw", [128, 128, 64], BF16)
    nm_in = dram_in(nc, "negMT", [256, S], BF16)
    oc_in = dram_in(nc, "oc", [64, S], BF16)
    g_in = dram_in(nc, "G3", [3, 64, S], BF16)
    et_in = dram_in(nc, "etab", [64, S], BF16)
    out = dram_out(nc, "onsa", [64, S], BF16)
    P = Prog(nc)
    fin = []
    KE = P.sb("KE", [128, S], BF16); dKE = Dep()
    KW = P.sb("KW", [64, S], BF16); dKW = Dep()
    for i in range(4):
        sl = slice(i * 4096, (i + 1) * 4096)
        P.dma(KE[0:64, sl], ks_in[:, sl], writes=[dKE])
        P.dma(KE[64:128, sl], et_in[:, sl], writes=[dKE])
        P.dma(KW[:, sl], kw_in[:, sl], writes=[dKW])
    VS = P.sb("VS", [128, 128, 128], BF16); dVS = Dep()
    VW = P.sb("VW", [128, 128, 128], BF16); dVW = Dep()
    P.op("pool", lambda E: E.memset(VS[:, :, 64:128], 1.0), writes=[dVS])
    P.op("pool", lambda E: E.memset(VW[:, :, 64:128], 1.0), writes=[dVW])
    for i in range(4):
        cs = slice(i * 32, (i + 1) * 32)
        P.dma(VS[:, cs, 0:64], vs_in[:, cs, :], writes=[dVS])
        P.dma(VW[:, cs, 0:64], vw_in[:, cs, :], writes=[dVW])
    qmr = Ring(P, "qm", 8, [128, 512], BF16)
    psr = Ring(P, "ps", 3, [128, 512], F32, psum=True)
    posr = Ring(P, "pos", 2, [128, 512], F32, psum=True)
    powr = Ring(P, "pow", 2, [128, 512], F32, psum=True)
    pr = Ring(P, "p", 4, [128, 512], BF16)
    gr = Ring(P, "g", 2, [64, 3, 512], BF16)
    ocr = Ring(P, "oc", 2, [64, 512], BF16)
    rr = Ring(P, "r", 2, [64, 512], F32)
    tsr = Ring(P, "ts", 2, [64, 512], F32)
    twr = Ring(P, "tw", 2, [64, 512], F32)
    obr = Ring(P, "ob", 2, [64, 512], BF16)

    def attend(pacc, dpacc, chunks, lhs_fn, rhs_fn, V, dV, g, win):
        n = len(chunks)

        def qk(kc):
            ps, dps = psr.next()
            lhsT, dl = lhs_fn(kc)
            rhs, drh = rhs_fn(kc)
            P.op("pe", lambda E: E.matmul(ps[:], lhsT=lhsT, rhs=rhs, start=True, stop=True), reads=[dl, drh], writes=[dps])
            p, dp = pr.next()
            P.op("act", lambda E: E.activation(out=p[:], in_=ps[:], func=AF.Exp), reads=[dps], writes=[dp])
            if kc >= 4 * g:
                P.op("pool", lambda E: E.affine_select(out=p[:], in_=p[:], pattern=[[1, 512]], compare_op=ALU.is_ge, fill=0.0,
                                                       base=-128 * (kc - 4 * g), channel_multiplier=-1), reads=[dp], writes=[dp])
            elif win:
                P.op("pool", lambda E: E.affine_select(out=p[:], in_=p[:], pattern=[[-1, 512]], compare_op=ALU.is_ge, fill=0.0,
                                                       base=128 * (kc - 4 * g) + 511, channel_multiplier=1), reads=[dp], writes=[dp])
            return p, dp

        cur = qk(chunks[0])
        for i, kc in enumerate(chunks):
            nxt = qk(chunks[i + 1]) if i + 1 < n else None
            p, dp = cur
            P.op("pe", lambda E: E.matmul(pacc[:], lhsT=V[:, kc, :], rhs=p[:], start=(i == 0), stop=(i == n - 1)), reads=[dV, dp], writes=[dpacc])
            cur = nxt

    for g in range(n_g):
        sl = slice(g * 512, (g + 1) * 512)
        nch = 4 * g + 4
        ngrp = (nch - 1) // 32 + 1
        qms = []
        for gi in range(ngrp):
            qm, dqm = qmr.next()
            P.dma(qm[0:64, :], qr_in[:, sl], writes=[dqm])
            P.dma(qm[64:128, :], nm_in[64 * gi:64 * gi + 64, sl], writes=[dqm])
            qms.append((qm, dqm))
        gt, dgt = gr.next()
        for b in range(3):
            P.dma(gt[:, b, :], g_in[b, :, sl], writes=[dgt])
        oct_, doc = ocr.next()
        P.dma(oct_[:], oc_in[:, sl], writes=[doc])
        pos_, dpos = posr.next()
        attend(pos_, dpos, list(range(nch)),
               lambda kc: (KE[:, kc * 128:(kc + 1) * 128], dKE),
               lambda kc: (qms[kc // 32][0][:], qms[kc // 32][1]), VS, dVS, g, False)
        pow_, dpow = powr.next()
        attend(pow_, dpow, list(range(max(0, 4 * g - 4), nch)),
               lambda kc: (KW[0:64, kc * 128:(kc + 1) * 128], dKW),
               lambda kc: (qms[0][0][0:64, :], qms[0][1]), VW, dVW, g, True)
        r1, dr1 = rr.next()
        ts, dts = tsr.next()
        P.op("dve", lambda E: E.reciprocal(out=r1[:], in_=pos_[64:128, :]), reads=[dpos], writes=[dr1])
        P.op("dve", lambda E: E.tensor_tensor(out=ts[:], in0=pos_[0:64, :], in1=r1[:], op=ALU.mult), reads=[dpos, dr1], writes=[dts])
        P.op("dve", lambda E: E.tensor_tensor(out=ts[:], in0=ts[:], in1=gt[:, 1, :], op=ALU.mult), reads=[dts, dgt], writes=[dts])
        r2, dr2 = rr.next()
        tw, dtw = twr.next()
        P.op("dve", lambda E: E.reciprocal(out=r2[:], in_=pow_[64:128, :]), reads=[dpow], writes=[dr2])
        P.op("dve", lambda E: E.tensor_tensor(out=tw[:], in0=pow_[0:64, :], in1=r2[:], op=ALU.mult), reads=[dpow, dr2], writes=[dtw])
        P.op("dve", lambda E: E.tensor_tensor(out=tw[:], in0=tw[:], in1=gt[:, 2, :], op=ALU.mult), reads=[dtw, dgt], writes=[dtw])
        P.op("dve", lambda E: E.tensor_tensor(out=ts[:], in0=ts[:], in1=tw[:], op=ALU.add), reads=[dts, dtw], writes=[dts])
        P.op("dve", lambda E: E.tensor_tensor(out=tw[:], in0=oct_[:], in1=gt[:, 0, :], op=ALU.mult), reads=[doc, dgt], writes=[dtw])
        ob, dob = obr.next()
        P.op("dve", lambda E: E.tensor_tensor(out=ob[:], in0=ts[:], in1=tw[:], op=ALU.add), reads=[dts, dtw], writes=[dob])
        dd = Dep()
        P.dma(out[:, sl], ob[:], reads=[dob], writes=[dd], prim=dob)
        fin.append(dob)
    st = P.finalize(final_deps=fin)
    return nc, st


def b1_consts(core):
    c = np.arange(1024)
    blk = np.arange(256)
    c_start = 16 * c[:, None]
    s_start = 64 * blk[None, :]
    ov = ((c_start < s_start + 64) & (c_start + 32 > s_start)).astype(np.float32)
    ov[1023, :] = 0.0
    ovl = np.concatenate([ov, np.ones((1024, 1), np.float32)], 1).reshape(8, 128, 257).transpose(1, 0, 2)
    t = core * TOK + np.arange(TOK)
    tq = np.broadcast_to(t[None, :].astype(np.float32), (128, TOK))
    cval = (16 * (128 * np.arange(8)[None, :] + np.arange(128)[:, None]) + 31).astype(np.float32)
    bi = np.broadcast_to(blk[None, :].astype(np.float32), (128, 256)).copy()
    bic = bi.copy(); bic[:, 0] = 1e9
    b0 = np.zeros((128, 256), np.float32); b0[:, 0] = 1.0
    cur = (t // 64).reshape(TOK // 128, 128).T.astype(np.float32)
    curt = np.stack([cur, cur - 1, cur - 2], 2)
    return dict(ovl=np.ascontiguousarray(ovl), tq=np.ascontiguousarray(tq), cval=cval, bi=bi, bic=bic, b0=b0,
                curt=np.ascontiguousarray(curt))


def layout_cmp_weights(cmp_pe, cmp_w1, cmp_w2):
    w1 = np.ascontiguousarray(cmp_w1.reshape(2, 16, 128, 256).transpose(0, 2, 1, 3))
    w2 = np.ascontiguousarray(cmp_w2.reshape(2, 2, 128, 64).transpose(0, 2, 1, 3))
    pef = np.ascontiguousarray(cmp_pe.reshape(2, 16, 2, 64).transpose(0, 2, 3, 1).reshape(2, 128, 16))
    return w1, w2, pef


def build_phase_b1():
    nc = bass.Bass("TRN2", target_bir_lowering=False)
    kv_in = dram_in(nc, "kcvc", [128, S + 16], BF16)
    qp_in = dram_in(nc, "qp", [8, 64, TOK], BF16)
    w1_in = dram_in(nc, "cw1", [2, 128, 16, 256], F32)
    w2_in = dram_in(nc, "cw2", [2, 128, 2, 64], F32)
    pe_in = dram_in(nc, "pef", [2, 128, 16], F32)
    ovl_in = dram_in(nc, "ovl", [128, 8, 257], F32)
    tq_in = dram_in(nc, "tq", [128, TOK], F32)
    cval_in = dram_in(nc, "cval", [128, 8], F32)
    bi_in = dram_in(nc, "bi", [128, 256], F32)
    bic_in = dram_in(nc, "bic", [128, 256], F32)
    b0_in = dram_in(nc, "b0", [128, 256], F32)
    curt_in = dram_in(nc, "curt", [128, 16, 3], F32)
    ident_in = dram_in(nc, "ident", [128, 128], F32)
    o_oc = dram_out(nc, "ocT", [8, 64, TOK], BF16)
    o_nm = dram_out(nc, "negMT", [256, TOK], BF16)
    P = Prog(nc)
    fin = []
    NT = TOK // 128

    def load_const(name, src, shape, dt=F32):
        t = P.sb(name, shape, dt); d = Dep()
        P.dma(t[:], src, writes=[d])
        return t, d
    tq, dtq = load_const("tq", tq_in, [128, TOK])
    cval, dcv = load_const("cval", cval_in, [128, 8])
    bi, dbi = load_const("bi", bi_in, [128, 256])
    bic, dbic = load_const("bic", bic_in, [128, 256])
    b0, db0 = load_const("b0", b0_in, [128, 256])
    curt, dcur = load_const("curt", curt_in, [128, 16, 3])
    idf, didf = load_const("idf", ident_in, [128, 128])
    ident = P.sb("ident", [128, 128], BF16); dident = Dep()
    P.op("dve", lambda E: E.tensor_copy(out=ident[:], in_=idf[:]), reads=[didf], writes=[dident])
    ovf, dovf = load_const("ovf", ovl_in, [128, 8, 257])
    ovl = P.sb("ovl", [128, 8, 257], BF16); dovl = Dep()
    P.op("dve", lambda E: E.tensor_copy(out=ovl[:], in_=ovf[:]), reads=[dovf], writes=[dovl])

    pacc = Ring(P, "pacc", 4, [128, 512], F32, psum=True)
    por = Ring(P, "po", 2, [128, 512], F32, psum=True)
    ptr = P.ps("ptr", [128, 256], BF16); dptr = Dep()
    kcT = P.sb("kcT", [64, 1024], BF16); dkcT = Dep()
    vca = P.sb("vca", [128, 8, 128], BF16); dvca = Dep()
    P.op("pool", lambda E: E.memset(vca[:], 1.0), writes=[dvca])

    P.open_scope()
    Dk = P.sb("Dk", [128, S], BF16); dDk = Dep()
    hid = P.sb("hid", [128, 2, 1024], BF16); dhid = Dep()
    w1f = P.sb("w1f", [128, 16, 256], F32); dw1f = Dep()
    w1b = P.sb("w1b", [128, 16, 256], BF16); dw1b = Dep()
    w2f = P.sb("w2f", [128, 2, 64], F32); dw2f = Dep()
    w2b = P.sb("w2b", [128, 2, 64], BF16); dw2b = Dep()
    pf = P.sb("pf", [128, 16], F32); dpf = Dep()
    pfb = P.sb("pfb", [128, 16], BF16); dpfb = Dep()
    bias = P.sb("bias", [128, 2], F32); dbias = Dep()
    Dv = Dk[:].rearrange("p (c s) -> p c s", s=16)
    for kv in range(2):
        rb = 64 * kv
        for i in range(4):
            sl = slice(i * 4096, (i + 1) * 4096)
            P.dma(Dk[0:64, sl], kv_in[rb:rb + 64, i * 4096:(i + 1) * 4096], writes=[dDk])
            P.dma(Dk[64:128, sl], kv_in[rb:rb + 64, i * 4096 + 1:(i + 1) * 4096 + 1], writes=[dDk])
        P.dma(w1f[:], w1_in[kv], writes=[dw1f])
        P.op("pool", lambda E: E.tensor_copy(out=w1b[:], in_=w1f[:]), reads=[dw1f], writes=[dw1b])
        P.dma(w2f[:], w2_in[kv], writes=[dw2f])
        P.op("dve", lambda E: E.tensor_copy(out=w2b[:], in_=w2f[:]), reads=[dw2f], writes=[dw2b])
        P.dma(pf[:], pe_in[kv], writes=[dpf])
        P.op("dve", lambda E: E.tensor_copy(out=pfb[:], in_=pf[:]), reads=[dpf], writes=[dpfb])
        P.op("dve", lambda E: E.memset(hid[:], 0.0), writes=[dhid])
        pb, dpb = pacc.next()
        for mc in range(2):
            for l2 in range(16):
                P.op("pe", lambda E: E.matmul(pb[:, mc:mc + 1], lhsT=w1b[:, l2, mc * 128:(mc + 1) * 128], rhs=pfb[:, l2:l2 + 1],
                                             start=(l2 == 0), stop=(l2 == 15)), reads=[dw1b, dpfb], writes=[dpb])
        P.op("dve", lambda E: E.tensor_copy(out=bias[:], in_=pb[:, 0:2]), reads=[dpb], writes=[dbias])
        for mc in range(2):
            for half in range(2):
                n = 512 if half == 0 else 511
                ph, dph = pacc.next()
                for l2 in range(16):
                    P.op("pe", lambda E: E.matmul(ph[:, 0:n], lhsT=w1b[:, l2, mc * 128:(mc + 1) * 128],
                                                 rhs=Dv[:, half * 512 + l2 // 8:half * 512 + l2 // 8 + n, 2 * (l2 % 8)], start=(l2 == 0), stop=(l2 == 15)),
                         reads=[dw1b, dDk], writes=[dph])
                P.op("act", lambda E: E.activation(out=hid[:, mc, half * 512:half * 512 + n], in_=ph[:, 0:n], func=AF.Gelu_apprx_tanh,
                                                   bias=bias[:, mc:mc + 1]), reads=[dph, dbias], writes=[dhid])
        if kv == 0:
            for half in range(2):
                pk, dpk = pacc.next()
                for mc in range(2):
                    P.op("pe", lambda E: E.matmul(pk[0:64, :], lhsT=w2b[:, mc, :], rhs=hid[:, mc, half * 512:(half + 1) * 512],
                                                 start=(mc == 0), stop=(mc == 1)), reads=[dw2b, dhid], writes=[dpk])
                P.op("act", lambda E: E.activation(out=kcT[:, half * 512:(half + 1) * 512], in_=pk[0:64, :], func=AF.Copy), reads=[dpk], writes=[dkcT])
        else:
            for cc in range(8):
                pv, dpv = pacc.next()
                for mc in range(2):
                    P.op("pe", lambda E: E.matmul(pv[:, 0:64], lhsT=hid[:, mc, cc * 128:(cc + 1) * 128], rhs=w2b[:, mc, :],
                                                 start=(mc == 0), stop=(mc == 1)), reads=[dw2b, dhid], writes=[dpv])
                P.op("act", lambda E: E.activation(out=vca[:, cc, 0:64], in_=pv[:, 0:64], func=AF.Copy), reads=[dpv], writes=[dvca])
    P.close_scope()

    impacc = P.sb("impacc", [128, NT, 256], F32); dimp = Dep()
    EM = P.sb("EM", [128, 8, 512], BF16); dEMs = [Dep() for _ in range(8)]
    qtr = Ring(P, "qt", 2, [64, 512], BF16)
    er = Ring(P, "e", 3, [128, 512], F32)
    rrr = Ring(P, "rr", 2, [64, 512], F32)
    obr = Ring(P, "ob", 2, [64, 512], BF16)
    rinv = Ring(P, "rinv", 2, [128, 1], F32)
    for j in range(NSB):
        sl = slice(j * 512, (j + 1) * 512)
        for h in range(8):
            qt, dqt = qtr.next()
            P.dma(qt[:], qp_in[h, :, sl], writes=[dqt])
            po, dpo = por.next()

            def qk(cc):
                ps, dps = pacc.next()
                P.op("pe", lambda E: E.matmul(ps[:], lhsT=kcT[:, cc * 128:(cc + 1) * 128], rhs=qt[:], start=True, stop=True), reads=[dkcT, dqt], writes=[dps])
                e, de = er.next()
                P.op("act", lambda E: E.activation(out=e[:], in_=ps[:], func=AF.Exp), reads=[dps], writes=[de])
                P.op("dve", lambda E: E.scalar_tensor_tensor(out=EM[:, cc, :], in0=tq[:, sl], scalar=cval[:, cc:cc + 1], in1=e[:],
                                                             op0=ALU.is_ge, op1=ALU.mult), reads=[dtq, dcv, de], writes=[dEMs[cc]])
            qk(0)
            for cc in range(8):
                if cc + 1 < 8:
                    qk(cc + 1)
                P.op("pe", lambda E: E.matmul(po[:], lhsT=vca[:, cc, :], rhs=EM[:, cc, :], start=(cc == 0), stop=(cc == 7)), reads=[dvca, dEMs[cc]], writes=[dpo])
            r_, dr_ = rrr.next()
            P.op("dve", lambda E: E.tensor_scalar(out=r_[:], in0=po[64:128, :], scalar1=1e-30, scalar2=None, op0=ALU.max), reads=[dpo], writes=[dr_])
            P.op("dve", lambda E: E.reciprocal(out=r_[:], in_=r_[:]), reads=[dr_], writes=[dr_])
            ob, dob = obr.next()
            P.op("dve", lambda E: E.tensor_tensor(out=ob[:], in0=po[0:64, :], in1=r_[:], op=ALU.mult), reads=[dpo, dr_], writes=[dob])
            dd = Dep()
            P.dma(o_oc[h, :, sl], ob[:], reads=[dob], writes=[dd], prim=dob)
            fin.append(dob)
            for q4 in range(4):
                a = 4 * j + q4
                pi, dpi = pacc.next()
                for cc in range(8):
                    P.op("pe", lambda E: E.matmul(pi[:, 0:257], lhsT=EM[:, cc, q4 * 128:(q4 + 1) * 128], rhs=ovl[:, cc, :], start=(cc == 0), stop=(cc == 7)),
                         reads=[dEMs[cc], dovl], writes=[dpi])
                ri, dri = rinv.next()
                P.op("dve", lambda E: E.tensor_scalar(out=ri[:], in0=pi[:, 256:257], scalar1=1e-30, scalar2=None, op0=ALU.max), reads=[dpi], writes=[dri])
                P.op("dve", lambda E: E.reciprocal(out=ri[:], in_=ri[:]), reads=[dri], writes=[dri])
                if h == 0:
                    P.op("dve", lambda E: E.tensor_scalar(out=impacc[:, a, :], in0=pi[:, 0:256], scalar1=ri[:, 0:1], scalar2=None, op0=ALU.mult),
                         reads=[dpi, dri], writes=[dimp])
                else:
                    P.op("dve", lambda E: E.scalar_tensor_tensor(out=impacc[:, a, :], in0=pi[:, 0:256], scalar=ri[:, 0:1], in1=impacc[:, a, :],
                                                                 op0=ALU.mult, op1=ALU.add), reads=[dpi, dri, dimp], writes=[dimp])
    m1 = P.sb("m1", [128, 256], F32); dm1 = Dep()
    sc = P.sb("sc", [128, 256], F32); dsc = Dep()
    v8 = P.sb("v8", [128, 8], F32); dv8 = Dep()
    Mt = P.sb("Mt", [128, 256], F32); dMt = Dep()
    nmr = Ring(P, "nm", 2, [128, 256], BF16)
    nmtr = Ring(P, "nmt", 2, [128, 2, 128], BF16)
    nm_v = o_nm.rearrange("(b p) t -> p b t", p=128)
    for a in range(NT):
        P.op("dve", lambda E: E.tensor_scalar(out=m1[:], in0=bic[:], scalar1=curt[:, a, 2:3], scalar2=None, op0=ALU.is_le), reads=[dbic, dcur], writes=[dm1])
        P.op("dve", lambda E: E.scalar_tensor_tensor(out=sc[:], in0=impacc[:, a, :], scalar=1.0, in1=m1[:], op0=ALU.add, op1=ALU.mult),
             reads=[dimp, dm1], writes=[dsc])
        P.op("dve", lambda E: E.max(out=v8[:], in_=sc[:]), reads=[dsc], writes=[dv8])
        P.op("dve", lambda E: E.tensor_scalar(out=v8[:, 4:5], in0=v8[:, 4:5], scalar1=0.5, scalar2=None, op0=ALU.max), reads=[dv8], writes=[dv8])
        P.op("dve", lambda E: E.tensor_scalar(out=Mt[:], in0=sc[:], scalar1=v8[:, 4:5], scalar2=None, op0=ALU.is_ge), reads=[dsc, dv8], writes=[dMt])
        P.op("dve", lambda E: E.scalar_tensor_tensor(out=Mt[:], in0=bi[:], scalar=curt[:, a, 0:1], in1=Mt[:], op0=ALU.is_equal, op1=ALU.add),
             reads=[dbi, dcur, dMt], writes=[dMt])
        P.op("dve", lambda E: E.scalar_tensor_tensor(out=Mt[:], in0=bi[:], scalar=curt[:, a, 1:2], in1=Mt[:], op0=ALU.is_equal, op1=ALU.add),
             reads=[dbi, dcur, dMt], writes=[dMt])
        P.op("dve", lambda E: E.tensor_tensor(out=Mt[:], in0=Mt[:], in1=b0[:], op=ALU.add), reads=[dMt, db0], writes=[dMt])
        P.op("dve", lambda E: E.tensor_scalar(out=Mt[:], in0=Mt[:], scalar1=1.0, scalar2=1.0, op0=ALU.min, op1=ALU.subtract), reads=[dMt], writes=[dMt])
        nm, dnm = nmr.next()
        P.op("dve", lambda E: E.tensor_scalar(out=nm[:], in0=Mt[:], scalar1=-NEG, scalar2=None, op0=ALU.mult), reads=[dMt], writes=[dnm])
        for b in range(2):
            P.op("pe", lambda E: E.transpose(ptr[:, b * 128:(b + 1) * 128], nm[:, b * 128:(b + 1) * 128], ident[:]), reads=[dnm, dident], writes=[dptr])
        nmt, dnmt = nmtr.next()
        P.op("act", lambda E: E.activation(out=nmt[:], in_=ptr[:].rearrange("p (b t) -> p b t", b=2), func=AF.Copy), reads=[dptr], writes=[dnmt])
        dd = Dep()
        P.dma(nm_v[:, :, a * 128:(a + 1) * 128], nmt[:], reads=[dnmt], writes=[dd], prim=dnmt)
        fin.append(dnmt)
    st = P.finalize(final_deps=fin)
    return nc, st


_PROGS = {}


def _prog(name, builder):
    nc, _ = builder()
    return nc


def _run(nc, maps):
    res = run_bass_kernel_spmd(nc, maps, core_ids=list(range(NCORES)))
    return res.results


def _bc(v, n=128):
    return np.ascontiguousarray(np.broadcast_to(np.asarray(v)[None, :], (n, v.shape[0])))


def kernel(x, positions, norm_g, w_in, cmp_pe, cmp_w1, cmp_w2, w_nsa_o, w_sb_o, w_out, w_ff1, w_ff2):
    import ml_dtypes
    bf = ml_dtypes.bfloat16
    x = np.asarray(x, np.float32)
    positions = np.asarray(positions)
    xs = [np.ascontiguousarray(x[0, c * TOK:(c + 1) * TOK]) for c in range(NCORES)]
    pos = [np.ascontiguousarray(np.broadcast_to(positions[0, c * TOK:(c + 1) * TOK][None, :].astype(np.int32), (128, TOK)))
           for c in range(NCORES)]
    ident = np.eye(128, dtype=np.float32)
    rc = rope_consts()
    etab = etab_const().astype(bf)
    tri = np.tril(np.ones((128, 128), np.float32)).astype(bf)
    ones = np.ones((128, 128), bf)
    mks = [sb_mask_table(p).astype(bf) for p in range(2)]
    b1c = [b1_consts(c) for c in range(NCORES)]
    cat = lambda key, rs, ax: np.concatenate([np.asarray(r[key]) for r in rs], axis=ax)
    for l in range(DEPTH):
        ng = np.asarray(norm_g[l], np.float32)
        fm, tm = layout_w_in(np.asarray(w_in[l], np.float32))
        gbc = _bc(ng[0])
        ra = _run(_prog("a", build_phase_a),
                  [dict(x=xs[c], pos=pos[c], gbc=gbc, rc=rc, ident=ident, wfm=fm, wtm=tm) for c in range(NCORES)])
        del fm, tm
        kcvc = np.zeros((128, S + 16), bf)
        kcvc[:, :S] = cat("kcvcT", ra, 1)
        w1, w2, pef = layout_cmp_weights(np.asarray(cmp_pe[l], np.float32), np.asarray(cmp_w1[l], np.float32),
                                         np.asarray(cmp_w2[l], np.float32))
        maps = []
        for c in range(NCORES):
            m = dict(kcvc=kcvc, qp=np.asarray(ra[c]["qpT"]), cw1=w1, cw2=w2, pef=pef, ident=ident)
            m.update(b1c[c])
            maps.append(m)
        rb1 = _run(_prog("b1", build_phase_b1), maps)
        qr_full = cat("qrT", ra, 2)
        kskw = cat("kskwT", ra, 1)
        vsvw = cat("vsvw", ra, 0)
        vs = np.ascontiguousarray(vsvw[:, 0:64].reshape(128, 128, 64).transpose(1, 0, 2))
        vw = np.ascontiguousarray(vsvw[:, 64:128].reshape(128, 128, 64).transpose(1, 0, 2))
        negMT = cat("negMT", rb1, 1)
        oc_full = cat("ocT", rb1, 2)
        G_full = cat("G", ra, 2)
        ks = np.ascontiguousarray(kskw[0:64]); kw = np.ascontiguousarray(kskw[64:128])
        rb2 = _run(_prog("b2", build_phase_b2),
                   [dict(qr=np.ascontiguousarray(qr_full[h]), ks=ks, kw=kw, vs=vs, vw=vw, negMT=negMT,
                         oc=np.ascontiguousarray(oc_full[h]), G3=np.ascontiguousarray(G_full[3 * h:3 * h + 3]), etab=etab)
                    for h in range(NCORES)])
        sq_full = cat("sqT", ra, 2)
        sk_full = cat("skT", ra, 2)
        sv_full = cat("sv", ra, 0)
        maps = []
        for c in range(NCORES):
            hh, par = c // 2, c % 2
            sq = np.stack([sq_full[hh][:, 512 * (2 * m + par):512 * (2 * m + par) + 512] for m in range(16)], 1)
            svh = np.ascontiguousarray(sv_full[:, 128 * hh:128 * hh + 128].reshape(128, 128, 128).transpose(1, 0, 2))
            maps.append(dict(sq=np.ascontiguousarray(sq), sk=np.ascontiguousarray(sk_full[hh]), sv=svh, mk=mks[par], tri=tri, ones=ones))
        rsb = _run(_prog("sb", build_phase_sb), maps)
        onsa_full = np.stack([np.asarray(rb2[h]["onsa"]) for h in range(NCORES)], 0)
        osb_full = np.zeros((4, 128, S), bf)
        for c in range(NCORES):
            hh, par = c // 2, c % 2
            o = np.asarray(rsb[c]["osb"])
            for m in range(16):
                g = 2 * m + par
                osb_full[hh][:, 512 * g:512 * g + 512] = o[:, m, :]
        wn, ws, wo, w1f, w2f = layout_c_weights(np.asarray(w_nsa_o[l], np.float32), np.asarray(w_sb_o[l], np.float32),
                                                np.asarray(w_out[l], np.float32), np.asarray(w_ff1[l], np.float32),
                                                np.asarray(w_ff2[l], np.float32))
        g123 = np.ascontiguousarray(np.broadcast_to(ng[1:4][:, None, :], (3, 128, D)))
        maps = []
        for c in range(NCORES):
            tsl = slice(c * TOK, (c + 1) * TOK)
            maps.append(dict(x=xs[c], onsaT=np.ascontiguousarray(onsa_full[:, :, tsl]), osbT=np.ascontiguousarray(osb_full[:, :, tsl]),
                             mgT=np.asarray(ra[c]["mgT"]), g123=g123, ident=ident, wn=wn, ws=ws, wo=wo, w1=w1f, w2=w2f))
        rc_ = _run(_prog("c", build_phase_c), maps)
        xs = [np.ascontiguousarray(np.asarray(rc_[c]["xo"], np.float32)) for c in range(NCORES)]
    out = np.concatenate(xs, 0)[None].astype(np.float32)
    return out
```
